# Optimizing a Trainium2 kernel written in Bass

```python
import jax
import jax.numpy as jnp
from jax import lax
import numpy as np

D_MODEL = 1024
BATCH = 16
SEQ = 4096
DEPTH = 2
DEC_BATCH = 16
DEC_SEQ = 64
PAST_LEN = 4096

CHUNK = 64
HEAD_DIM = 64
N_SB_HEADS = 8
N_FOX_HEADS = 8
N_SWA_HEADS = 16
N_SWA_KV_HEADS = 2
SWA_GROUP = N_SWA_HEADS // N_SWA_KV_HEADS
WINDOW = 128
WINDOW_CHUNKS = WINDOW // CHUNK
BAND = (WINDOW_CHUNKS + 1) * CHUNK
SWA_CACHE = WINDOW
QUERY_BLOCK = 128
D_FF = 2816
N_EXPERTS = 8
TOP_K = 2
D_EXPERT = 3584
MOE_BLOCK = 256
N_AB_LAYERS = (DEPTH + 1) // 2
N_C_LAYERS = DEPTH // 2
SB_W = N_SB_HEADS * HEAD_DIM
FOX_W = N_FOX_HEADS * HEAD_DIM
MIX_AB_IN = 3 * SB_W + 3 * FOX_W + N_FOX_HEADS
SWA_Q_W = N_SWA_HEADS * HEAD_DIM
SWA_KV_W = N_SWA_KV_HEADS * HEAD_DIM
MIX_C_IN = SWA_Q_W + 2 * SWA_KV_W
FORGET_BIAS_MEAN = 2.0
RMS_EPS = 1e-6
NEG_INF = -1e30
ATTN_SCALE = HEAD_DIM ** -0.5

kernel_name = 'hybrid_stream_encoder_step'


def rms_norm(x, g):
    x32 = x.astype(jnp.float32)
    y = x32 * lax.rsqrt(jnp.mean(x32 * x32, axis=-1, keepdims=True) + RMS_EPS)
    return (y * g.astype(jnp.float32)).astype(x.dtype)


def swiglu(x, w_gate, w_up, w_down):
    return (jax.nn.silu(x @ w_gate) * (x @ w_up)) @ w_down


def alibi_slopes(n_heads):
    return 2.0 ** (-8.0 * (jnp.arange(n_heads, dtype=jnp.float32) + 1.0) / n_heads)


def to_blocks(a, blk=QUERY_BLOCK):
    b, t = a.shape[:2]
    return jnp.moveaxis(a.reshape((b, t // blk, blk) + a.shape[2:]), 1, 0)


def from_blocks(a):
    a = jnp.moveaxis(a, 0, 1)
    return a.reshape((a.shape[0], a.shape[1] * a.shape[2]) + a.shape[3:])


def stick_breaking_attend(q, k, v, q_pos, k_pos):
    z = jnp.einsum('bqhd,bkhd->bhqk', q.astype(jnp.float32), k.astype(jnp.float32)) * ATTN_SCALE
    vis = k_pos[None, :] < q_pos[:, None]
    log_keep = jnp.where(vis, jax.nn.log_sigmoid(-z), 0.0)
    log_keep_after = lax.cumsum(log_keep, axis=3, reverse=True) - log_keep
    w = jnp.where(vis, jnp.exp(jax.nn.log_sigmoid(z) + log_keep_after), 0.0)
    return jnp.einsum('bhqk,bkhd->bqhd', w, v.astype(jnp.float32)).astype(v.dtype)


def forgetting_attend(q, k, v, c_q, c_k, q_pos, k_pos):
    s = jnp.einsum('bqhd,bkhd->bhqk', q.astype(jnp.float32), k.astype(jnp.float32)) * ATTN_SCALE
    s = s + jnp.swapaxes(c_q, 1, 2)[:, :, :, None] - jnp.swapaxes(c_k, 1, 2)[:, :, None, :]
    vis = k_pos[None, :] <= q_pos[:, None]
    p = jax.nn.softmax(jnp.where(vis, s, NEG_INF), axis=-1)
    return jnp.einsum('bhqk,bkhd->bqhd', p, v.astype(jnp.float32)).astype(v.dtype)


def swa_attend(q, k, v, q_pos, k_pos, sinks):
    s = jnp.einsum('...qhgd,...khd->...hgqk', q.astype(jnp.float32), k.astype(jnp.float32)) * ATTN_SCALE
    tq = q_pos[..., :, None]
    sk = k_pos[..., None, :]
    vis = (sk >= 0) & (sk // CHUNK <= tq // CHUNK) & (sk // CHUNK >= tq // CHUNK - WINDOW_CHUNKS)
    dist = jnp.abs(tq - sk).astype(jnp.float32)
    slopes = alibi_slopes(N_SWA_HEADS).reshape(N_SWA_KV_HEADS, SWA_GROUP, 1, 1)
    s = s - slopes * dist[..., None, None, :, :]
    s = jnp.where(vis[..., None, None, :, :], s, NEG_INF)
    sink = jnp.broadcast_to(sinks.astype(jnp.float32).reshape(N_SWA_KV_HEADS, SWA_GROUP, 1, 1), s.shape[:-1] + (1,))
    p = jax.nn.softmax(jnp.concatenate([s, sink], axis=-1), axis=-1)[..., :-1]
    return jnp.einsum('...hgqk,...khd->...qhgd', p, v.astype(jnp.float32)).astype(v.dtype)


def mix_ab_project(h, w_in, b_forget):
    b, t = h.shape[:2]
    p = h @ w_in
    q_sb, k_sb, v_sb, q_fx, k_fx, v_fx, f_logit = jnp.split(
        p, [SB_W, 2 * SB_W, 3 * SB_W, 3 * SB_W + FOX_W, 3 * SB_W + 2 * FOX_W, 3 * SB_W + 3 * FOX_W], axis=-1)
    sb = lambda a: a.reshape(b, t, N_SB_HEADS, HEAD_DIM)
    fx = lambda a: a.reshape(b, t, N_FOX_HEADS, HEAD_DIM)
    logf = jax.nn.log_sigmoid((f_logit + b_forget).astype(jnp.float32))
    return sb(q_sb), sb(k_sb), sb(v_sb), fx(q_fx), fx(k_fx), fx(v_fx), logf


def mixer_ab(h, past, past_len, w_in, b_forget, w_out):
    b, t = h.shape[:2]
    q_sb, k_sb, v_sb, q_fx, k_fx, v_fx, logf = mix_ab_project(h, w_in, b_forget)
    if past is None:
        pos = jnp.arange(t)
        c = jnp.cumsum(logf, axis=1)

        def block(args):
            qs, qf, cq, qp = args
            return (stick_breaking_attend(qs, k_sb, v_sb, qp, pos),
                    forgetting_attend(qf, k_fx, v_fx, cq, c, qp, pos))

        o_sb, o_fx = lax.map(block, (to_blocks(q_sb), to_blocks(q_fx), to_blocks(c), pos.reshape(-1, QUERY_BLOCK)))
        o_sb, o_fx = from_blocks(o_sb), from_blocks(o_fx)
    else:
        p_sb_k, p_sb_v, p_fx_k, p_fx_v, p_logf = past
        k_pos = jnp.arange(past_len + t)
        q_pos = past_len + jnp.arange(t)
        c = jnp.cumsum(jnp.concatenate([p_logf.astype(jnp.float32), logf], axis=1), axis=1)
        o_sb = stick_breaking_attend(q_sb, jnp.concatenate([p_sb_k, k_sb], axis=1),
                                     jnp.concatenate([p_sb_v, v_sb], axis=1), q_pos, k_pos)
        o_fx = forgetting_attend(q_fx, jnp.concatenate([p_fx_k, k_fx], axis=1),
                                 jnp.concatenate([p_fx_v, v_fx], axis=1), c[:, past_len:], c, q_pos, k_pos)
    o = jnp.concatenate([o_sb, o_fx], axis=2).reshape(b, t, SB_W + FOX_W)
    return o @ w_out, (k_sb, v_sb, k_fx, v_fx, logf)


def mixer_c(h, past, past_len, w_in, sinks, w_out):
    b, t = h.shape[:2]
    q, k, v = jnp.split(h @ w_in, [SWA_Q_W, SWA_Q_W + SWA_KV_W], axis=-1)
    q = q.reshape(b, t, N_SWA_KV_HEADS, SWA_GROUP, HEAD_DIM)
    k = k.reshape(b, t, N_SWA_KV_HEADS, HEAD_DIM)
    v = v.reshape(b, t, N_SWA_KV_HEADS, HEAD_DIM)
    if past is None:
        nc = t // CHUNK

        def band(a):
            a = a.reshape(b, nc, CHUNK, N_SWA_KV_HEADS, HEAD_DIM)
            a = jnp.concatenate([jnp.zeros((b, WINDOW_CHUNKS, CHUNK, N_SWA_KV_HEADS, HEAD_DIM), a.dtype), a], axis=1)
            return jnp.concatenate([a[:, j:j + nc] for j in range(WINDOW_CHUNKS + 1)], axis=2)

        chunk_start = jnp.arange(nc)[:, None] * CHUNK
        q_pos = chunk_start + jnp.arange(CHUNK)[None, :]
        k_pos = chunk_start + (jnp.arange(BAND) - WINDOW_CHUNKS * CHUNK)[None, :]
        qc = q.reshape(b, nc, CHUNK, N_SWA_KV_HEADS, SWA_GROUP, HEAD_DIM)
        o = lax.map(lambda a: swa_attend(a[0], a[1], a[2], q_pos, k_pos, sinks), (qc, band(k), band(v)))
        new_k, new_v = k[:, t - SWA_CACHE:], v[:, t - SWA_CACHE:]
    else:
        p_k, p_v = past
        k_all = jnp.concatenate([p_k, k], axis=1)
        v_all = jnp.concatenate([p_v, v], axis=1)
        q_pos = past_len + jnp.arange(t)
        k_pos = past_len - SWA_CACHE + jnp.arange(SWA_CACHE + t)
        o = swa_attend(q, k_all, v_all, q_pos, k_pos, sinks)
        new_k, new_v = k_all[:, t:], v_all[:, t:]
    return o.reshape(b, t, SWA_Q_W) @ w_out, (new_k, new_v)


def moe_swiglu(x, w_router, w_gate, w_up, w_down):
    shape = x.shape
    d = shape[-1]
    xf = x.reshape(-1, d)
    n_tok = xf.shape[0]
    logits = (xf @ w_router).astype(jnp.float32)
    top_logits, top_idx = lax.top_k(logits, TOP_K)
    gates = jax.nn.softmax(top_logits, axis=-1).reshape(-1)
    expert = top_idx.reshape(-1)
    token = jnp.repeat(jnp.arange(n_tok, dtype=jnp.int32), TOP_K)
    n_assign = n_tok * TOP_K
    order = jnp.argsort(expert)
    expert_sorted = expert[order]
    counts = jnp.bincount(expert, length=N_EXPERTS)
    starts = jnp.cumsum(counts) - counts
    padded = (counts + MOE_BLOCK - 1) // MOE_BLOCK * MOE_BLOCK
    padded_end = jnp.cumsum(padded)
    padded_start = padded_end - padded
    dest = padded_start[expert_sorted] + jnp.arange(n_assign) - starts[expert_sorted]
    n_blocks = -(-n_assign // MOE_BLOCK) + N_EXPERTS
    cap = n_blocks * MOE_BLOCK
    slot_token = jnp.full((cap,), n_tok, jnp.int32).at[dest].set(token[order])
    slot_gate = jnp.zeros((cap,), jnp.float32).at[dest].set(gates[order])
    block_expert = jnp.minimum(jnp.searchsorted(padded_end, jnp.arange(n_blocks) * MOE_BLOCK, side='right'), N_EXPERTS - 1)
    x_pad = jnp.concatenate([xf, jnp.zeros((1, d), xf.dtype)], axis=0)
    x_slots = x_pad[slot_token].reshape(n_blocks, MOE_BLOCK, d)

    def expert_block(args):
        xb, e = args
        return swiglu(xb, w_gate[e], w_up[e], w_down[e])

    y_slots = lax.map(expert_block, (x_slots, block_expert)).reshape(cap, d)
    y_slots = (y_slots.astype(jnp.float32) * slot_gate[:, None]).astype(x.dtype)
    y = jnp.zeros((n_tok + 1, d), x.dtype).at[slot_token].add(y_slots)
    return y[:n_tok].reshape(shape)


def setup_inputs(seed: int = 0) -> dict:
    key = jax.random.key(seed)
    ks = jax.random.split(key, 27)
    d = D_MODEL

    def nrm(i, shape, scale=1.0):
        return scale * jax.random.normal(ks[i], shape, jnp.float32)

    return {
        'x_prompt': nrm(0, (BATCH, SEQ, d)),
        'x_sample': nrm(1, (DEC_BATCH, DEC_SEQ, d)),
        'cache_sb_k': nrm(2, (N_AB_LAYERS, DEC_BATCH, PAST_LEN, N_SB_HEADS, HEAD_DIM)),
        'cache_sb_v': nrm(3, (N_AB_LAYERS, DEC_BATCH, PAST_LEN, N_SB_HEADS, HEAD_DIM)),
        'cache_fox_k': nrm(4, (N_AB_LAYERS, DEC_BATCH, PAST_LEN, N_FOX_HEADS, HEAD_DIM)),
        'cache_fox_v': nrm(5, (N_AB_LAYERS, DEC_BATCH, PAST_LEN, N_FOX_HEADS, HEAD_DIM)),
        'cache_fox_logf': jax.nn.log_sigmoid(FORGET_BIAS_MEAN + nrm(6, (N_AB_LAYERS, DEC_BATCH, PAST_LEN, N_FOX_HEADS), 0.5)),
        'cache_swa_k': nrm(7, (N_C_LAYERS, DEC_BATCH, SWA_CACHE, N_SWA_KV_HEADS, HEAD_DIM)),
        'cache_swa_v': nrm(8, (N_C_LAYERS, DEC_BATCH, SWA_CACHE, N_SWA_KV_HEADS, HEAD_DIM)),
        'norm_mix_ab': 1.0 + nrm(9, (N_AB_LAYERS, d), 0.02),
        'w_in_ab': nrm(10, (N_AB_LAYERS, d, MIX_AB_IN), d ** -0.5),
        'b_forget': FORGET_BIAS_MEAN + nrm(11, (N_AB_LAYERS, N_FOX_HEADS), 0.1),
        'w_out_ab': nrm(12, (N_AB_LAYERS, SB_W + FOX_W, d), (SB_W + FOX_W) ** -0.5),
        'norm_ffn_dense': 1.0 + nrm(13, (N_AB_LAYERS, d), 0.02),
        'w_gate_dense': nrm(14, (N_AB_LAYERS, d, D_FF), d ** -0.5),
        'w_up_dense': nrm(15, (N_AB_LAYERS, d, D_FF), d ** -0.5),
        'w_down_dense': nrm(16, (N_AB_LAYERS, D_FF, d), D_FF ** -0.5),
        'norm_mix_c': 1.0 + nrm(17, (N_C_LAYERS, d), 0.02),
        'w_in_c': nrm(18, (N_C_LAYERS, d, MIX_C_IN), d ** -0.5),
        'sinks': nrm(19, (N_C_LAYERS, N_SWA_HEADS), 0.5),
        'w_out_c': nrm(20, (N_C_LAYERS, SWA_Q_W, d), SWA_Q_W ** -0.5),
        'norm_ffn_moe': 1.0 + nrm(21, (N_C_LAYERS, d), 0.02),
        'w_router': nrm(22, (N_C_LAYERS, d, N_EXPERTS), d ** -0.5),
        'w_gate_moe': nrm(23, (N_C_LAYERS, N_EXPERTS, d, D_EXPERT), d ** -0.5),
        'w_up_moe': nrm(24, (N_C_LAYERS, N_EXPERTS, d, D_EXPERT), d ** -0.5),
        'w_down_moe': nrm(25, (N_C_LAYERS, N_EXPERTS, D_EXPERT, d), D_EXPERT ** -0.5),
        'norm_final': 1.0 + nrm(26, (d,), 0.02),
    }


def reference(x_prompt, x_sample, cache_sb_k, cache_sb_v, cache_fox_k, cache_fox_v, cache_fox_logf,
              cache_swa_k, cache_swa_v, norm_mix_ab, w_in_ab, b_forget, w_out_ab, norm_ffn_dense,
              w_gate_dense, w_up_dense, w_down_dense, norm_mix_c, w_in_c, sinks, w_out_c, norm_ffn_moe,
              w_router, w_gate_moe, w_up_moe, w_down_moe, norm_final):
    past_len = cache_sb_k.shape[2]

    def run(x, use_cache):
        ab_states, c_states = [], []
        for layer in range(DEPTH):
            i = layer // 2
            if layer % 2 == 0:
                past = (cache_sb_k[i], cache_sb_v[i], cache_fox_k[i], cache_fox_v[i], cache_fox_logf[i]) if use_cache else None
                o, st = mixer_ab(rms_norm(x, norm_mix_ab[i]), past, past_len, w_in_ab[i], b_forget[i], w_out_ab[i])
                x = x + o
                x = x + swiglu(rms_norm(x, norm_ffn_dense[i]), w_gate_dense[i], w_up_dense[i], w_down_dense[i])
                ab_states.append(st)
            else:
                past = (cache_swa_k[i], cache_swa_v[i]) if use_cache else None
                o, st = mixer_c(rms_norm(x, norm_mix_c[i]), past, past_len, w_in_c[i], sinks[i], w_out_c[i])
                x = x + o
                x = x + moe_swiglu(rms_norm(x, norm_ffn_moe[i]), w_router[i], w_gate_moe[i], w_up_moe[i], w_down_moe[i])
                c_states.append(st)
        return rms_norm(x, norm_final), ab_states, c_states

    y_prompt, p_ab, p_c = run(x_prompt, False)
    y_sample, s_ab, s_c = run(x_sample, True)

    def stack(states, j):
        return jnp.stack([st[j] for st in states])

    return (y_prompt, y_sample,
            stack(p_ab, 0), stack(p_ab, 1), stack(p_ab, 2), stack(p_ab, 3), stack(p_ab, 4), stack(p_c, 0), stack(p_c, 1),
            stack(s_ab, 0), stack(s_ab, 1), stack(s_ab, 2), stack(s_ab, 3), stack(s_ab, 4), stack(s_c, 0), stack(s_c, 1))
```

```python
import contextlib
import numpy as np
import ml_dtypes
import concourse.bass as bass
import concourse.mybir as mybir
from concourse.bass_utils import run_bass_kernel_spmd

F32 = mybir.dt.float32
BF16NP = ml_dtypes.bfloat16
BF16 = mybir.dt.bfloat16
I32 = mybir.dt.int32
AF = mybir.ActivationFunctionType
ALU = mybir.AluOpType

D = 1024
NQ = 64
HD = 64
D_FF = 2816
D_EXP = 3584
NEXP = 8
MIX_AB_IN = 3080
MIX_C_IN = 1280
NEGBIG = -30000.0
EPS = 1e-6
COMPUTE = ("pe", "act", "dve", "pool")


class Op:
    __slots__ = ("eng", "fn", "deps", "is_dma", "seq", "ring", "ringval", "has_dependents")

    def __init__(self, eng, fn, is_dma=False):
        self.eng = eng
        self.fn = fn
        self.deps = []
        self.is_dma = is_dma
        self.has_dependents = False
        self.seq = None
        self.ring = None
        self.ringval = None


class Buf:
    __slots__ = ("name", "writers", "readers", "multi", "excl")

    def __init__(self, name="", multi=False, excl=False):
        self.name = name
        self.writers = []
        self.readers = []
        self.multi = multi
        self.excl = excl


class Prog:
    EP = 30000
    REP = 2000

    def __init__(self, nc, n_ring=8):
        self.nc = nc
        self.ops = {e: [] for e in ("pe", "act", "dve", "pool", "sync")}
        self.n_ring = n_ring
        self.n_ops = 0

    def add(self, eng, fn, reads=(), writes=(), is_dma=False):
        op = Op(eng, fn, is_dma)
        deps = set()
        for b in reads:
            deps.update(b.writers)
            if b.excl:
                deps.update(r for r in b.readers if r.eng != eng)
        for b in writes:
            if not b.multi:
                deps.update(b.writers)
                deps.update(b.readers)
        for b in reads:
            if not b.multi:
                b.readers.append(op)
        for b in writes:
            if b.multi:
                b.writers.append(op)
            else:
                b.writers = [op]
                b.readers = []
        deps.discard(op)
        for d in deps:
            if d.eng == "pe" and eng == "pe" and not d.is_dma and not is_dma:
                continue
            op.deps.append(d)
            d.has_dependents = True
        self.ops[eng].append(op)
        self.n_ops += 1
        return op

    def barrier(self):
        lasts = []
        for e in COMPUTE:
            for op in reversed(self.ops[e]):
                if not op.is_dma and op.fn is not None:
                    lasts.append(op)
                    break
        for q in ("sync", "pool"):
            k = 0
            for op in reversed(self.ops[q]):
                if op.is_dma:
                    lasts.append(op)
                    k += 1
                    if k >= self.n_ring:
                        break
        for e in self.ops:
            op = Op(e, None)
            for d in lasts:
                op.deps.append(d)
                d.has_dependents = True
            self.ops[e].append(op)

    def emit(self):
        nc = self.nc
        self.barrier()
        EP = self.EP
        REP = self.REP
        with contextlib.ExitStack() as st:
            nep = {}
            for e in COMPUTE:
                c = 0
                for op in self.ops[e]:
                    if op.is_dma or op.fn is None:
                        continue
                    if op.has_dependents:
                        op.seq = (c // EP, c % EP + 1)
                        c += 1
                nep[e] = max(1, (c + EP - 1) // EP)
            sems = {e: [st.enter_context(nc.semaphore(f"s_{e}_{i}")) for i in range(nep[e])] for e in COMPUTE}
            rings = {}
            for q in ("sync", "pool"):
                k = 0
                for op in self.ops[q]:
                    if op.is_dma:
                        use = k // self.n_ring
                        op.ring = (use // REP, k % self.n_ring)
                        op.ringval = (16 * (use % REP + 1), use)
                        k += 1
                nuse = (k + self.n_ring - 1) // self.n_ring
                nrep = max(1, (nuse + REP - 1) // REP)
                rings[q] = [[st.enter_context(nc.semaphore(f"r_{q}_{j}_{i}")) for i in range(self.n_ring)] for j in range(nrep)]
            block = st.enter_context(nc.Block())

            def run_stream(e, eng):
                waited = {}

                def wait(key, sem, val):
                    if waited.get(key, 0) >= val:
                        return
                    waited[key] = val
                    eng.wait_ge(sem, val)

                for op in self.ops[e]:
                    for d in op.deps:
                        if d.is_dma:
                            ep, slot = d.ring
                            wait(("r", d.eng, ep, slot), rings[d.eng][ep][slot], d.ringval[0])
                        else:
                            ep, v = d.seq
                            wait(("c", d.eng, ep), sems[d.eng][ep], v)
                    if op.fn is None:
                        continue
                    if op.is_dma:
                        ep, slot = op.ring
                        val, use = op.ringval
                        if use > 0:
                            pu = use - 1
                            wait(("r", e, pu // REP, slot), rings[e][pu // REP][slot], 16 * (pu % REP + 1))
                        op.fn(eng).then_inc(rings[e][ep][slot], 16)
                    else:
                        ins = op.fn(eng)
                        if op.has_dependents:
                            ins.then_inc(sems[e][op.seq[0]], 1)

            @block.tensor
            def _(eng):
                run_stream("pe", eng)

            @block.scalar
            def _(eng):
                run_stream("act", eng)

            @block.vector
            def _(eng):
                run_stream("dve", eng)

            @block.gpsimd
            def _(eng):
                run_stream("pool", eng)

            @block.sync
            def _(eng):
                run_stream("sync", eng)


class Arena:
    def __init__(self, t, nbytes):
        self.t = t
        self.nbytes = nbytes
        self.off = 0
        self.base = 0

    def reset(self):
        self.off = self.base

    def alloc(self, free_shape, dt, parts=128):
        esz = 4 if dt in (F32, I32) else 2
        n = int(np.prod(free_shape))
        nb = (n * esz + 63) // 64 * 64
        assert self.off + nb <= self.nbytes, ("SBUF arena overflow", self.off, nb, self.nbytes)
        v = self.t[0:parts, self.off // 2:(self.off + n * esz) // 2]
        self.off += nb
        if esz == 4:
            v = v.bitcast(dt)
        if len(free_shape) == 2:
            v = v.rearrange("p (a b) -> p a b", a=free_shape[0])
        elif len(free_shape) == 3:
            v = v.rearrange("p (a b c) -> p a b c", a=free_shape[0], b=free_shape[1])
        return v, Buf()


def build(SEQ, PAST, stop_after=None):
    nc = bass.Bass("TRN2", target_bir_lowering=False)
    P = Prog(nc)
    NTP = SEQ // 128
    NTOK = 2 * SEQ + 2 * NQ
    NT = NTOK // 128
    TKS = PAST + NQ
    NKBC = PAST // 128

    def din(name, shape, dt=F32):
        return nc.dram_tensor(name, list(shape), dt, kind="ExternalInput").ap()

    def dout(name, shape, dt=F32):
        return nc.dram_tensor(name, list(shape), dt, kind="ExternalOutput").ap()

    def dscr(name, shape, dt):
        return nc.dram_tensor(name, list(shape), dt, kind="Internal").ap(), Buf(name, multi=True)

    xp = din("xp", [2, SEQ, D]); xs = din("xs", [2, NQ, D])
    c_sbk = din("c_sbk", [2, PAST, 512]); c_sbv = din("c_sbv", [2, PAST, 512])
    c_fxk = din("c_fxk", [2, PAST, 512]); c_fxv = din("c_fxv", [2, PAST, 512])
    c_lf = din("c_lf", [2, PAST, 8])
    c_wk = din("c_wk", [2, 128, 128]); c_wv = din("c_wv", [2, 128, 128])
    g_ab = din("g_ab", [1, D]); w_in_ab = din("w_in_ab", [D, MIX_AB_IN]); b_fg = din("b_fg", [1, 8])
    w_out_ab = din("w_out_ab", [D, D]); g_ffn = din("g_ffn", [1, D])
    w_g = din("w_g", [D, D_FF]); w_u = din("w_u", [D, D_FF]); w_d = din("w_d", [D_FF, D])
    g_c = din("g_c", [1, D]); w_in_c = din("w_in_c", [D, MIX_C_IN]); sinks = din("sinks", [1, 16])
    w_out_c = din("w_out_c", [D, D]); g_moe = din("g_moe", [1, D]); w_r = din("w_r", [D, NEXP])
    w_ge = din("w_ge", [NEXP, D, D_EXP]); w_ue = din("w_ue", [NEXP, D, D_EXP]); w_de = din("w_de", [NEXP, D_EXP, D])
    g_fin = din("g_fin", [1, D])
    cst = din("cst", [128, 14, 128])
    alibi = din("alibi", [128, 16, 256])
    alibi2 = din("alibi2", [128, 2, 16, 256])
    iot = din("iot", [128, 64])

    y_p = dout("y_p", [2, SEQ, D]); y_s = dout("y_s", [2, NQ, D])
    o_p = {k: dout("p_" + k, [2, SEQ, 512]) for k in ("sbk", "sbv", "fxk", "fxv")}
    o_plf = dout("p_lf", [2, SEQ, 8])
    o_pwk = dout("p_wk", [2, 128, 128]); o_pwv = dout("p_wv", [2, 128, 128])
    o_s = {k: dout("s_" + k, [2, NQ, 512]) for k in ("sbk", "sbv", "fxk", "fxv")}
    o_slf = dout("s_lf", [2, NQ, 8])
    o_swk = dout("s_wk", [2, 128, 128]); o_swv = dout("s_wv", [2, 128, 128])
    OUTB = Buf("outs", multi=True)

    qt_p, B_qt_p = dscr("qt_p", [2, 2, 8, 64, SEQ], BF16)
    kt_p, B_kt_p = dscr("kt_p", [2, 2, 8, 64, SEQ], BF16)
    v_p, B_v_p = dscr("v_p", [2, 2, SEQ, 512], BF16)
    qt_s, B_qt_s = dscr("qt_s", [2, 2, 8, 64, NQ], BF16)
    kt_s, B_kt_s = dscr("kt_s", [2, 2, 8, 64, TKS], BF16)
    v_s, B_v_s = dscr("v_s", [2, 2, NQ, 512], BF16)
    c_p, B_c_p = dscr("c_p", [2, SEQ, 8], F32); ct_p, B_ct_p = dscr("ct_p", [2, 8, SEQ], F32)
    c_s, B_c_s = dscr("c_s", [2, TKS, 8], F32); ct_s, B_ct_s = dscr("ct_s", [2, 8, TKS], F32)
    ao_p, B_ao_p = dscr("ao_p", [2, 16, 64, SEQ], BF16)
    ao_s, B_ao_s = dscr("ao_s", [2, 16, 64, NQ], BF16)
    x1, B_x1 = dscr("x1", [NTOK, D], F32)
    h2t, B_h2t = dscr("h2t", [8, 128, NTOK], BF16)
    actt, B_actt = dscr("actt", [22, 128, NTOK], BF16)
    x2, B_x2 = dscr("x2", [NTOK, D], F32)
    qct, B_qct = dscr("qct", [16, 64, NTOK], BF16)
    kct, B_kct = dscr("kct", [2, 64, NTOK], BF16)
    vc, B_vc = dscr("vc", [NTOK, 128], BF16)
    x3, B_x3 = dscr("x3", [NTOK, D], F32)
    x4 = nc.dram_tensor("x4", [NTOK, D], F32, kind="Internal").ap()
    ao_c, B_aoc = dscr("aoc", [16, 64, NTOK], BF16)
    h4t, B_h4t = dscr("h4t", [8, 128, NTOK], BF16)
    h4k, B_h4k = dscr("h4k", [NTOK, D], BF16)
    NBLK = (2 * NTOK + NEXP * 511) // 512
    CAP = NBLK * 512
    assert NBLK <= 48
    xsl, B_xsl = dscr("xsl", [CAP, D], BF16)
    ysl, B_ysl = dscr("ysl", [CAP, D], F32)
    wge_s, B_wge = dscr("wge_s", [NEXP * 4 * 128, 8 * 896], BF16)
    wue_s, B_wue = dscr("wue_s", [NEXP * 4 * 128, 8 * 896], BF16)
    wde_s, B_wde = dscr("wde_s", [NEXP * 4 * 128, 7 * 1024], BF16)

    st = contextlib.ExitStack()
    ARB = 196 * 1024
    art = st.enter_context(nc.sbuf_tensor("arena", [128, ARB // 2], BF16))
    ar = Arena(art, ARB)
    pbanks = [st.enter_context(nc.psum_tensor(f"pb{i}", [128, 512], F32)) for i in range(8)]
    PB = [Buf(f"pb{i}", excl=True) for i in range(8)]

    def dma(q, out, in_, reads=(), writes=(), **kw):
        return P.add(q, lambda e, o=out, i=in_, k=kw: e.dma_start(out=o, in_=i, **k), reads, writes, is_dma=True)

    def mm(out, lhsT, rhs, start, stop, reads, writes):
        return P.add("pe", lambda e, o=out, l=lhsT, r=rhs, s=start, t=stop: e.matmul(o, lhsT=l, rhs=r, start=s, stop=t),
                     reads, writes)

    def tr(out, in_, ident, reads, writes):
        return P.add("pe", lambda e, o=out, i=in_, d=ident: e.transpose(out=o, in_=i, identity=d), reads, writes)

    def act(out, in_, func, reads, writes, bias=None, scale=None, accum=None):
        kw = {}
        if bias is not None:
            kw["bias"] = bias
        if scale is not None:
            kw["scale"] = scale
        if accum is not None:
            kw["accum_out"] = accum
        return P.add("act", lambda e, o=out, i=in_, f=func, k=kw: e.activation(out=o, in_=i, func=f, **k), reads, writes)

    def tt(eng, out, in0, in1, op, reads, writes):
        return P.add(eng, lambda e, o=out, a=in0, b=in1, p=op: e.tensor_tensor(out=o, in0=a, in1=b, op=p), reads, writes)

    def ts(eng, out, in0, s1, op0, reads, writes, s2=None, op1=None):
        if op1 is None:
            return P.add(eng, lambda e, o=out, a=in0, s=s1, p=op0: e.tensor_scalar(out=o, in0=a, scalar1=s, scalar2=None, op0=p),
                         reads, writes)
        return P.add(eng, lambda e, o=out, a=in0, s=s1, p=op0, s_2=s2, p1=op1:
                     e.tensor_scalar(out=o, in0=a, scalar1=s, scalar2=s_2, op0=p, op1=p1), reads, writes)

    def stt(out, in0, scalar, in1, op0, op1, reads, writes):
        return P.add("dve", lambda e, o=out, a=in0, s=scalar, b=in1, p0=op0, p1=op1:
                     e.scalar_tensor_tensor(out=o, in0=a, scalar=s, in1=b, op0=p0, op1=p1), reads, writes)

    def cp(eng, out, in_, reads, writes):
        if eng == "act":
            return act(out, in_, AF.Copy, reads, writes)
        return P.add(eng, lambda e, o=out, i=in_: e.tensor_copy(out=o, in_=i), reads, writes)

    def memset(eng, ap, val, writes):
        return P.add(eng, lambda e, a=ap, v=val: e.memset(a, v), (), writes)

    cf, B_cf = ar.alloc([14, 128], F32)
    cb, B_cb = ar.alloc([14, 128], BF16)
    dma("sync", cf, cst, writes=[B_cf])
    cp("dve", cb, cf, [B_cf], [B_cb])
    (C_ID, C_NUI, C_VSB, C_VFX, C_MSB, C_MFX, C_TRI, C_ONE, C_OLO, C_OHI, C_TRS, C_N64, C_TR2, C_SEL0) = range(14)
    iota_t, B_iota = ar.alloc([64], F32)
    dma("sync", iota_t, iot, writes=[B_iota])
    MK1, B_MK1 = ar.alloc([NT, NEXP], F32)
    MK2, B_MK2 = ar.alloc([NT, NEXP], F32)
    RANK, B_RK = ar.alloc([NT, NEXP], F32)
    G12, B_G12 = ar.alloc([NT, 2], F32)
    spm, spmB = ar.alloc([NEXP], F32)
    SL, B_SL = ar.alloc([NT, 2], I32)
    ar.base = ar.off
    CB = [B_cf, B_cb, B_iota]

    gtile = {}

    def load_gain(name, src):
        t, b = ar.alloc([D], F32)
        dma("sync", t, src[0:1, :].partition_broadcast(128).rearrange("p o d -> p (o d)"), writes=[b])
        gtile[name] = (t, b)

    def rmsnorm_to_bf16(x_ap, xb, g, hb, hbB, nrows, tmp):
        (sq, sqB), (ss, ssB), (rs, rsB) = tmp
        act(sq[0:nrows], x_ap, AF.Square, [xb], [sqB, ssB], accum=ss[0:nrows])
        act(rs[0:nrows], ss[0:nrows], AF.Ln, [ssB], [rsB], scale=1.0 / D, bias=EPS)
        act(rs[0:nrows], rs[0:nrows], AF.Exp, [rsB], [rsB], scale=-0.5)
        stt(hb, x_ap, rs[0:nrows, 0:1], g[0][0:nrows], ALU.mult, ALU.mult, [xb, rsB, g[1]], [hbB])

    def transpose_to(hb, hbB, dstT, dstB, col0, nrows, pbi):
        ptb = pbanks[pbi][:].bitcast(BF16)
        for kc in range(8):
            tr(ptb[:, kc * 128:kc * 128 + nrows], hb[0:nrows, kc * 128:(kc + 1) * 128], cb[0:nrows, C_ID, 0:nrows],
               [hbB, B_cb], [PB[pbi]])
        cp("act", dstT[:, :, col0:col0 + nrows], ptb.rearrange("p (k t) -> p k t", k=8)[:, :, 0:nrows], [PB[pbi]], [dstB])

    def x_rows(seq, t0, n):
        return xp[seq, t0:t0 + n, :]

    tiles = []
    for sq in range(2):
        for t0 in range(0, SEQ, 512):
            tiles.append(("p", sq, t0, 512, sq * SEQ + t0))
    tiles.append(("s", 0, 0, 128, 2 * SEQ))

    ar.reset()
    win, B_win = ar.alloc([8, 3136], BF16)
    for kc in range(8):
        dma("pool", win[:, kc, 0:MIX_AB_IN], w_in_ab[kc * 128:(kc + 1) * 128, :], writes=[B_win])
    load_gain("ab", g_ab)
    bfg, B_bfg = ar.alloc([8], F32)
    dma("sync", bfg, b_fg[0:1, :].partition_broadcast(128).rearrange("p o d -> p (o d)"), writes=[B_bfg])
    if stop_after == 'A0':
        P.barrier()
        return nc, P, st, locals()
    xt2 = [ar.alloc([4, D], F32) for _ in range(2)]
    hb_a = [ar.alloc([D], BF16) for _ in range(2)]
    hT_a = [ar.alloc([8, 512], BF16) for _ in range(2)]
    tmp_a = (ar.alloc([D], F32), ar.alloc([1], F32), ar.alloc([1], F32))
    qkst = [ar.alloc([512], BF16) for _ in range(4)]
    kvst = [ar.alloc([512], F32) for _ in range(4)]
    vbst = [ar.alloc([512], BF16) for _ in range(2)]
    sm = {k: ar.alloc([8], F32) for k in ("f", "e", "l", "lf", "c", "sp0", "sp1", "sps0", "sps1")}
    ctst = [ar.alloc([128], F32, parts=8) for _ in range(2)]
    ktst = [ar.alloc([512], BF16) for _ in range(2)]
    cach = [ar.alloc([512], BF16) for _ in range(2)]
    clft = [ar.alloc([8], F32) for _ in range(2)]

    def cumsum_tile(lf, lfB, nrows, sprev_list, dst_c, dst_cB, dst_ct, dst_ctB, i, sample_pair=False):
        pc = 6
        tri = cf[:, C_TR2 if sample_pair else C_TRI, :]
        ones = [cf[:, C_ONE, :]] if not sample_pair else [cf[:, C_OLO, :], cf[:, C_OHI, :]]
        mm(pbanks[pc][0:128, 0:8], tri, lf, True, False, [B_cf, lfB], [PB[pc]])
        for j, (sp, spB) in enumerate(sprev_list):
            mm(pbanks[pc][0:128, 0:8], ones[j], sp, False, j == len(sprev_list) - 1, [B_cf, spB], [PB[pc]])
        mm(pbanks[pc][0:8, 128:256], lf, tri, True, False, [B_cf, lfB], [PB[pc]])
        for j, (sp, spB) in enumerate(sprev_list):
            mm(pbanks[pc][0:8, 128:256], sp, ones[j], False, j == len(sprev_list) - 1, [B_cf, spB], [PB[pc]])
        ct, ctB = sm["c"]
        cp("dve", ct, pbanks[pc][0:128, 0:8], [PB[pc]], [ctB])
        ctt, cttB = ctst[i % 2]
        cp("dve", ctt, pbanks[pc][0:8, 128:256], [PB[pc]], [cttB])
        for (d_ap, rows) in dst_c:
            dma("sync", d_ap, ct[rows[0]:rows[1], :], reads=[ctB], writes=[dst_cB])
        for (d_ap, cols) in dst_ct:
            dma("sync", d_ap, ctt[:, cols[0]:cols[1]], reads=[cttB], writes=[dst_ctB])

    for sq in range(2):
        memset("dve", sm["sps%d" % sq][0], 0.0, [sm["sps%d" % sq][1]])

    def cache_step(sq, kb):
        sp, spB = sm["sps%d" % sq]
        if True:
            for ki, (csrc) in enumerate((c_sbk, c_fxk)):
                i = (kb * 2 + ki)
                ct_, cB_ = cach[i % 2]
                dma("pool", ct_, csrc[sq, kb * 128:(kb + 1) * 128, :], writes=[cB_])
                ptb = pbanks[7][:].bitcast(BF16)
                for j in range(4):
                    tr(ptb[:, j * 128:(j + 1) * 128], ct_[:, j * 128:(j + 1) * 128], cb[:, C_ID, :], [cB_, B_cb], [PB[7]])
                kt_, kB_ = ktst[i % 2]
                cp("act" if i % 2 else "dve", kt_, ptb[:, 0:512], [PB[7]], [kB_])
                for j in range(4):
                    dma("sync", kt_s[sq, ki, 2 * j:2 * j + 2, :, kb * 128:(kb + 1) * 128].rearrange("h d t -> (h d) t"),
                        kt_[:, j * 128:(j + 1) * 128], reads=[kB_], writes=[B_kt_s])
            lt, lB = clft[kb % 2]
            dma("sync", lt, c_lf[sq, kb * 128:(kb + 1) * 128, :], writes=[lB])
            cumsum_tile(lt, lB, 128, [(sp, spB)], [(c_s[sq, kb * 128:(kb + 1) * 128, :], (0, 128))], B_c_s,
                        [(ct_s[sq, :, kb * 128:(kb + 1) * 128], (0, 128))], B_ct_s, kb)
            tt("dve", sp, sp, lt, ALU.add, [spB, lB], [spB])

    cache_steps = [(sq_, kb_) for sq_ in range(2) for kb_ in range(NKBC)]
    n_ptiles = sum(1 for t_ in tiles if t_[0] == "p")
    cs_per = -(-len(cache_steps) // n_ptiles)
    cs_done = [0]

    def emit_cache_steps(n):
        for _ in range(n):
            if cs_done[0] < len(cache_steps):
                cache_step(*cache_steps[cs_done[0]])
                cs_done[0] += 1

    if stop_after == 'A1':
        P.barrier()
        return nc, P, st, locals()
    import os as _os
    DBG = _os.environ.get("DBG", "").split(",")
    def a_load(ti):
        kind, sq, t0, TW, g0 = tiles[ti]
        xt, xB = xt2[ti % 2]
        if kind == "p":
            dma("sync", xt[:, 0:TW // 128, :], xp[sq, t0:t0 + TW, :].rearrange("(s p) d -> p s d", p=128), writes=[xB])
        else:
            dma("sync", xt[0:64, 0, :], xs[0], writes=[xB])
            dma("sync", xt[64:128, 0, :], xs[1], writes=[xB])

    def a_norm(ti):
        kind, sq, t0, TW, g0 = tiles[ti]
        xt, xB = xt2[ti % 2]
        hT, hTB = hT_a[ti % 2]
        for s in range(TW // 128):
            hb, hbB = hb_a[s % 2]
            rmsnorm_to_bf16(xt[:, s, :], xB, gtile["ab"], hb, hbB, 128, tmp_a)
            transpose_to(hb, hbB, hT, hTB, s * 128, 128, 7)

    a_load(0)
    if len(tiles) > 1:
        a_load(1)
    a_norm(0)
    for ti, (kind, sq, t0, TW, g0) in enumerate(tiles):
        nsub = TW // 128
        xt, xB = xt2[ti % 2]
        hT, hTB = hT_a[ti % 2]
        if ti + 1 < len(tiles):
            a_norm(ti + 1)
        if ti + 2 < len(tiles):
            a_load(ti + 2)
        if kind == "p" and t0 == 0:
            memset("dve", sm["sp0"][0], 0.0, [sm["sp0"][1]])
        emit_cache_steps(cs_per if kind == "p" else len(cache_steps))
        fm = [("q", 0, 0), ("k", 0, 512), ("q", 1, 1536), ("k", 1, 2048)]
        ei = 0
        for (qk, sbfx, col0) in fm:
            if "fm" in DBG:
                break
            for j in range(4):
                pbi = ei % 4
                for kc in range(8):
                    mm(pbanks[pbi][:, 0:TW], win[:, kc, col0 + j * 128:col0 + (j + 1) * 128], hT[:, kc, 0:TW],
                       kc == 0, kc == 7, [B_win, hTB], [PB[pbi]])
                stt_, stB = qkst[ei % 4]
                if qk == "q":
                    act(stt_[:, 0:TW], pbanks[pbi][:, 0:TW], AF.Copy, [PB[pbi]], [stB], scale=0.125)
                else:
                    cp("dve", stt_[:, 0:TW], pbanks[pbi][:, 0:TW], [PB[pbi]], [stB])
                if kind == "p":
                    dstt, dB = (qt_p, B_qt_p) if qk == "q" else (kt_p, B_kt_p)
                    dma("sync", dstt[sq, sbfx, 2 * j:2 * j + 2, :, t0:t0 + TW].rearrange("h d t -> (h d) t"),
                        stt_[:, 0:TW], reads=[stB], writes=[dB])
                else:
                    for s2 in range(2):
                        if qk == "q":
                            dma("sync", qt_s[s2, sbfx, 2 * j:2 * j + 2, :, :].rearrange("h d t -> (h d) t"),
                                stt_[:, s2 * 64:(s2 + 1) * 64], reads=[stB], writes=[B_qt_s])
                        else:
                            dma("sync", kt_s[s2, sbfx, 2 * j:2 * j + 2, :, PAST:PAST + NQ].rearrange("h d t -> (h d) t"),
                                stt_[:, s2 * 64:(s2 + 1) * 64], reads=[stB], writes=[B_kt_s])
                ei += 1
        for s in range(nsub):
            for bi, (nm, col0, isv, sbfx) in enumerate((("sbk", 512, False, 0), ("sbv", 1024, True, 0),
                                                        ("fxk", 2048, False, 1), ("fxv", 2560, True, 1))):
                if "tm" in DBG:
                    break
                pbi = 4 + bi % 2
                for kc in range(8):
                    mm(pbanks[pbi][:, :], hT[:, kc, s * 128:(s + 1) * 128], win[:, kc, col0:col0 + 512],
                       kc == 0, kc == 7, [B_win, hTB], [PB[pbi]])
                kvt, kvB = kvst[bi]
                cp("act" if bi % 2 else "dve", kvt, pbanks[pbi][:, :], [PB[pbi]], [kvB])
                if isv:
                    vbt, vbB = vbst[bi // 2]
                    cp("dve" if bi % 2 else "act", vbt, pbanks[pbi][:, :], [PB[pbi]], [vbB])
                if kind == "p":
                    dma("sync", o_p[nm][sq, t0 + s * 128:t0 + (s + 1) * 128, :], kvt, reads=[kvB], writes=[OUTB])
                    if isv:
                        dma("sync", v_p[sq, sbfx, t0 + s * 128:t0 + (s + 1) * 128, :], vbt, reads=[vbB], writes=[B_v_p])
                else:
                    for s2 in range(2):
                        dma("sync", o_s[nm][s2], kvt[s2 * 64:(s2 + 1) * 64, :], reads=[kvB], writes=[OUTB])
                        if isv:
                            dma("sync", v_s[s2, sbfx], vbt[s2 * 64:(s2 + 1) * 64, :], reads=[vbB], writes=[B_v_s])
            if "fg" in DBG:
                continue
            for kc in range(8):
                mm(pbanks[6][:, 256:264], hT[:, kc, s * 128:(s + 1) * 128], win[:, kc, 3072:3080], kc == 0, kc == 7,
                   [B_win, hTB], [PB[6]])
            (f_, fB), (e_, eB), (l_, lB), (lf_, lfB) = sm["f"], sm["e"], sm["l"], sm["lf"]
            tt("dve", f_, pbanks[6][:, 256:264], bfg, ALU.add, [PB[6], B_bfg], [fB])
            act(e_, f_, AF.Exp, [fB], [eB], scale=-1.0)
            act(l_, e_, AF.Ln, [eB], [lB], bias=1.0)
            ts("dve", lf_, l_, -1.0, ALU.mult, [lB], [lfB])
            if kind == "p":
                r0 = t0 + s * 128
                dma("sync", o_plf[sq, r0:r0 + 128, :], lf_, reads=[lfB], writes=[OUTB])
                sp, spB = sm["sp0"]
                cumsum_tile(lf_, lfB, 128, [(sp, spB)], [(c_p[sq, r0:r0 + 128, :], (0, 128))], B_c_p,
                            [(ct_p[sq, :, r0:r0 + 128], (0, 128))], B_ct_p, s)
                tt("dve", sp, sp, lf_, ALU.add, [spB, lfB], [spB])
            else:
                for s2 in range(2):
                    dma("sync", o_slf[s2], lf_[s2 * 64:(s2 + 1) * 64, :], reads=[lfB], writes=[OUTB])
                cumsum_tile(lf_, lfB, 128, [sm["sps0"], sm["sps1"]],
                            [(c_s[0, PAST:PAST + NQ, :], (0, 64)), (c_s[1, PAST:PAST + NQ, :], (64, 128))], B_c_s,
                            [(ct_s[0, :, PAST:PAST + NQ], (0, 64)), (ct_s[1, :, PAST:PAST + NQ], (64, 128))], B_ct_s, s,
                            sample_pair=True)
    P.barrier()
    if stop_after == 'A':
        return nc, P, st, locals()

    ar.reset()
    TKM = max(SEQ, TKS)
    NKM = TKM // 128 + 1
    ktA = [ar.alloc([TKM], BF16) for _ in range(2)]
    ktF = [ar.alloc([TKM], BF16) for _ in range(2)]
    qtA = [ar.alloc([SEQ], BF16) for _ in range(2)]
    qtF = [ar.alloc([SEQ], BF16) for _ in range(2)]
    qrowB = [Buf() for _ in range(2)]
    vA = [ar.alloc([NKM, 64], BF16) for _ in range(2)]
    vF = [ar.alloc([NKM, 128], BF16) for _ in range(2)]
    crow = [ar.alloc([SEQ], F32) for _ in range(2)]
    cref = [ar.alloc([8], F32) for _ in range(2)]
    ctok, B_ctok = ar.alloc([NKM, 8], F32)
    nbias = [ar.alloc([NKM], F32) for _ in range(2)]
    e_t = [ar.alloc([512], F32) for _ in range(2)]
    l_t = [ar.alloc([512], F32) for _ in range(2)]
    lp_t = [ar.alloc([512], BF16) for _ in range(2)]
    w_t = [ar.alloc([512], BF16) for _ in range(2)]
    aost = [ar.alloc([512], BF16) for _ in range(2)]
    rD_t, B_rD = ar.alloc([512], F32)
    zt, B_zt = ar.alloc([512], BF16)
    r32, B_r32 = ar.alloc([512], F32)
    memset("dve", zt, 0.0, [B_zt])
    for i in range(2):
        memset("dve", ktA[i][0][64:65, :], 1.0, [ktA[i][1]])
        memset("dve", ktF[i][0][64:65, :], 1.0, [ktF[i][1]])
        memset("pool", vF[i][0][:, :, 64:128], 1.0, [vF[i][1]])
    wf_t = [ar.alloc([512], BF16) for _ in range(2)]
    pAs = [0, 6]; pAf = 1; pBk = [2, 3]; pO = 4; pR = 5; pOf = 7

    def zero_bank(pbi, rows, ncols):
        mm(pbanks[pbi][0:rows, 0:ncols], zt[:, 0:rows], zt[:, 0:ncols], True, False, [B_zt], [PB[pbi]])

    def seq_params(sidx):
        is_p = sidx < 2
        sq = sidx % 2
        if is_p:
            groups = []
            for qb in range(SEQ // 512):
                tl = [(4 * qb + j, 128, 128 * j, True) for j in (3, 2, 1, 0)] + [(kb, 128, 0, False) for kb in range(4 * qb - 1, -1, -1)]
                groups.append((512 * qb, 512, 512 * qb, tl))
            return dict(is_p=True, sq=sq, QTs=qt_p[sq], KTs=kt_p[sq], Cs=c_p[sq], CTs=ct_p[sq], AOs=ao_p[sq], TQ=SEQ, TK=SEQ,
                        BQ=B_qt_p, BK=B_kt_p, BC=B_c_p, BCT=B_ct_p, BAO=B_ao_p, groups=groups, nkb_all=SEQ // 128)
        groups = [(0, NQ, PAST, [(NKBC, 64, 0, True)] + [(kb, 128, 0, False) for kb in range(NKBC - 1, -1, -1)])]
        return dict(is_p=False, sq=sq, QTs=qt_s[sq], KTs=kt_s[sq], Cs=c_s[sq], CTs=ct_s[sq], AOs=ao_s[sq], TQ=NQ, TK=TKS,
                    BQ=B_qt_s, BK=B_kt_s, BC=B_c_s, BCT=B_ct_s, BAO=B_ao_s, groups=groups, nkb_all=NKBC)

    def load_head(sp, h, a2):
        (kA, kAB), (kF, kFB), (qA, qAB), (qF, qFB) = ktA[a2], ktF[a2], qtA[a2], qtF[a2]
        (vAt, vAB), (vFt, vFB), (cr, crB), (crf, crfB) = vA[a2], vF[a2], crow[a2], cref[a2]
        sq, TK, TQ, KTs, QTs, CTs = sp["sq"], sp["TK"], sp["TQ"], sp["KTs"], sp["QTs"], sp["CTs"]
        BK, BQ, BCT, nkb_all = sp["BK"], sp["BQ"], sp["BCT"], sp["nkb_all"]
        dma("sync", kA[0:64, 0:TK], KTs[0, h], reads=[BK], writes=[kAB])
        dma("sync", kF[0:64, 0:TK], KTs[1, h], reads=[BK], writes=[kFB])
        dma("sync", qA[0:64, 0:TQ], QTs[0, h], reads=[BQ], writes=[qAB])
        dma("sync", qF[0:64, 0:TQ], QTs[1, h], reads=[BQ], writes=[qFB])
        if sp["is_p"]:
            for (vt, vB, ki) in ((vAt, vAB, 0), (vFt, vFB, 1)):
                dma("sync", vt[:, 0:nkb_all, 0:64], v_p[sq, ki, :, h * 64:(h + 1) * 64].rearrange("(k p) d -> p k d", p=128),
                    reads=[B_v_p], writes=[vB])
            dma("sync", cr[64:65, 0:SEQ], CTs[h:h + 1, :], reads=[BCT], writes=[crB])
            dma("sync", crf[:, 0:SEQ // 512],
                CTs[h:h + 1, 0:SEQ:512].partition_broadcast(128).rearrange("p o n -> p (o n)"), reads=[BCT], writes=[crfB],
                allow_slow_non_contiguous=True)
        else:
            for (vt, vB, ki, csrc) in ((vAt, vAB, 0, c_sbv), (vFt, vFB, 1, c_fxv)):
                dma("pool", vt[:, 0:NKBC, 0:64], csrc[sq, :, h * 64:(h + 1) * 64].rearrange("(k p) d -> p k d", p=128), writes=[vB])
                dma("sync", vt[0:64, NKBC, 0:64], v_s[sq, ki, :, h * 64:(h + 1) * 64], reads=[B_v_s], writes=[vB])
            dma("sync", cr[64:65, 0:NQ], CTs[h:h + 1, PAST:PAST + NQ], reads=[BCT], writes=[crB])
            dma("sync", crf[:, 0:1], CTs[h:h + 1, PAST:PAST + 1].partition_broadcast(128).rearrange("p o n -> p (o n)"),
                reads=[BCT], writes=[crfB])

    stg = [ar.alloc([7168], BF16) for _ in range(2)]
    chunks = [(e, q, mat) for e in range(NEXP) for q in range(4) for mat in range(3)]

    def precast_chunk(ci):
        e, q, mat = chunks[ci]
        t, b = stg[ci % 2]
        if mat < 2:
            src = (w_ge, w_ue)[mat][e].rearrange("(k p) m -> p k m", p=128)[:, :, q * 896:(q + 1) * 896]
            dma("pool", t[:, 0:7168].rearrange("p (k m) -> p k m", k=8), src, writes=[b])
            dst, dB = (wge_s, wue_s)[mat], (B_wge, B_wue)[mat]
        else:
            src = w_de[e, q * 896:(q + 1) * 896, :].rearrange("(m p) n -> p m n", p=128)
            dma("pool", t[:, 0:7168].rearrange("p (m n) -> p m n", m=7), src, writes=[b])
            dst, dB = wde_s, B_wde
        r0 = (e * 4 + q) * 128
        dma("sync", dst[r0:r0 + 128, :], t[:, 0:7168], reads=[b], writes=[dB])

    heads = [(sidx, h) for sidx in range(4) for h in range(8)]
    sps = [seq_params(sidx) for sidx in range(4)]
    aoc = 0
    load_head(sps[0], 0, 0)
    for hi, (sidx, h) in enumerate(heads):
        sp = sps[sidx]
        is_p, sq, Cs, AOs, BC, BAO, groups, nkb_all = sp["is_p"], sp["sq"], sp["Cs"], sp["AOs"], sp["BC"], sp["BAO"], sp["groups"], sp["nkb_all"]
        if h == 0:
            dma("sync", ctok[:, 0:nkb_all, :], Cs[0:nkb_all * 128, :].rearrange("(k p) h -> p k h", p=128), reads=[BC], writes=[B_ctok])
            if not is_p:
                dma("sync", ctok[0:64, NKBC, :], Cs[PAST:PAST + NQ, :], reads=[BC], writes=[B_ctok])
        if hi + 1 < len(heads):
            load_head(sps[heads[hi + 1][0]], heads[hi + 1][1], (hi + 1) % 2)
        for ci in range(3 * hi, 3 * hi + 3):
            precast_chunk(ci)
        a2 = hi % 2
        (kA, kAB), (kF, kFB), (qA, qAB), (qF, qFB) = ktA[a2], ktF[a2], qtA[a2], qtF[a2]
        (vAt, vAB), (vFt, vFB), (cr, crB), (crf, crfB) = vA[a2], vF[a2], crow[a2], cref[a2]
        qrB = qrowB[a2]
        for gi, (q0, ncols, cq0, tl) in enumerate(groups):
            n = len(tl)

            def tpar(i):
                kb, nk, c0, diag = tl[i]
                return kb, nk, c0, diag, slice(kb * 128, kb * 128 + nk), slice(q0 + c0, q0 + ncols), slice(c0, ncols)

            zero_bank(pO, 64, ncols)
            memset("dve", qA[64:65, q0:q0 + ncols], 0.0, [qrB])
            memset("pool", r32[64:65, 0:ncols], 0.0, [B_r32])
            zero_bank(pOf, 128, ncols)
            cq = cq0 if is_p else 0
            ts("dve", qF[64:65, q0:q0 + ncols], cr[64:65, cq:cq + ncols], cr[64:65, cq:cq + 1], ALU.subtract, [crB], [qFB])
            (nb, nbB) = nbias[gi % 2]
            ts("dve", nb[:, 0:nkb_all + 1], ctok[:, 0:nkb_all + 1, h], crf[:, gi:gi + 1], ALU.subtract, [B_ctok, crfB], [nbB],
               s2=-1.0, op1=ALU.mult)

            def sb_A(i):
                kb, nk, c0, diag, ks, qs, cs = tpar(i)
                a = i % 2
                (et, eB), (lt, lB), (lpt, lpB) = e_t[a], l_t[a], lp_t[a]
                mm(pbanks[pAs[i % 2]][0:nk, cs], kA[0:64, ks], qA[0:64, qs], True, True, [kAB, qAB], [PB[pAs[i % 2]]])
                act(et[0:nk, cs], pbanks[pAs[i % 2]][0:nk, cs], AF.Exp, [PB[pAs[i % 2]]], [eB], scale=-1.0)
                act(lt[0:nk, cs], et[0:nk, cs], AF.Ln, [eB], [lB], bias=1.0)
                tt("dve", lpt[0:nk, cs], pbanks[pAs[i % 2]][0:nk, cs], lt[0:nk, cs], ALU.add, [PB[pAs[i % 2]], lB], [lpB])
                if diag:
                    tt("dve", lpt[0:nk, c0:c0 + nk], lpt[0:nk, c0:c0 + nk], cb[0:nk, C_VSB, 0:nk], ALU.mult, [lpB, B_cb], [lpB])

            def sb_R(i):
                kb, nk, c0, diag, ks, qs, cs = tpar(i)
                (lpt, lpB) = lp_t[i % 2]
                mm(pbanks[pR][0:65, cs], cb[0:nk, C_N64, 0:65], lpt[0:nk, cs], True, True, [B_cb, lpB], [PB[pR]])
                tt("dve", r32[64:65, cs], pbanks[pR][64:65, cs], r32[64:65, cs], ALU.add, [PB[pR], B_r32], [B_r32])

            def sb_B(i):
                kb, nk, c0, diag, ks, qs, cs = tpar(i)
                a = i % 2
                (lpt, lpB), (wt, wB) = lp_t[a], w_t[a]
                mm(pbanks[pBk[a]][0:nk, cs], kA[0:65, ks], qA[0:65, qs], True, False, [kAB, qAB, qrB], [PB[pBk[a]]])
                if diag:
                    mm(pbanks[pBk[a]][0:nk, c0:c0 + nk], cb[0:nk, C_ID, 0:nk], cb[0:nk, C_MSB, 0:nk], False, False, [B_cb], [PB[pBk[a]]])
                mm(pbanks[pBk[a]][0:nk, cs], cb[0:nk, C_NUI, 0:nk], lpt[0:nk, cs], False, True, [B_cb, lpB], [PB[pBk[a]]])
                act(wt[0:nk, cs], pbanks[pBk[a]][0:nk, cs], AF.Exp, [PB[pBk[a]]], [wB])
                if i + 1 < n:
                    cp("pool", qA[64:65, q0 + c0:q0 + ncols], r32[64:65, cs], [B_r32], [qrB])

            def sb_O(i):
                kb, nk, c0, diag, ks, qs, cs = tpar(i)
                (wt, wB) = w_t[i % 2]
                mm(pbanks[pO][0:64, cs], vAt[0:nk, kb, :], wt[0:nk, cs], False, i + 1 == n, [vAB, wB], [PB[pO]])

            def fx_A(i):
                kb, nk, c0, diag, ks, qs, cs = tpar(i)
                (wt, wB) = wf_t[i % 2]
                mm(pbanks[pAf][0:nk, cs], kF[0:65, ks], qF[0:65, qs], True, not diag, [kFB, qFB], [PB[pAf]])
                if diag:
                    mm(pbanks[pAf][0:nk, c0:c0 + nk], cb[0:nk, C_ID, 0:nk], cb[0:nk, C_MFX, 0:nk], False, True, [B_cb], [PB[pAf]])
                act(wt[0:nk, cs], pbanks[pAf][0:nk, cs], AF.Exp, [PB[pAf], nbB], [wB], bias=nb[0:nk, kb:kb + 1])

            def fx_OD(i):
                kb, nk, c0, diag, ks, qs, cs = tpar(i)
                (wt, wB) = wf_t[i % 2]
                mm(pbanks[pOf][0:128, cs], vFt[0:nk, kb, :], wt[0:nk, cs], False, i + 1 == n, [vFB, wB], [PB[pOf]])

            for k in range(n + 2):
                if k < n:
                    sb_A(k)
                    fx_A(k)
                if 0 <= k - 2 < n:
                    sb_O(k - 2)
                if 0 <= k - 1 < n:
                    fx_OD(k - 1)
                    if k < n:
                        sb_R(k - 1)
                    sb_B(k - 1)
            (ao, aoB) = aost[aoc % 2]; aoc += 1
            cp("act", ao[0:64, 0:ncols], pbanks[pO][0:64, 0:ncols], [PB[pO]], [aoB])
            dma("sync", AOs[h, :, q0:q0 + ncols], ao[0:64, 0:ncols], reads=[aoB], writes=[BAO])
            P.add("dve", lambda e, o=rD_t[64:128, 0:ncols], i_=pbanks[pOf][64:128, 0:ncols]: e.reciprocal(out=o, in_=i_), [PB[pOf]], [B_rD])
            (ao, aoB) = aost[aoc % 2]; aoc += 1
            tt("dve", ao[0:64, 0:ncols], pbanks[pOf][0:64, 0:ncols], rD_t[64:128, 0:ncols], ALU.mult, [PB[pOf], B_rD], [aoB])
            dma("sync", AOs[8 + h, :, q0:q0 + ncols], ao[0:64, 0:ncols], reads=[aoB], writes=[BAO])
    P.barrier()
    if stop_after == 'B':
        return nc, P, st, locals()

    AX = mybir.AxisListType.X

    def reduce_max(out, in_, reads, writes):
        return P.add("dve", lambda e, o=out, i=in_: e.tensor_reduce(out=o, in_=i, axis=AX, op=ALU.max), reads, writes)

    def recip(out, in_, reads, writes):
        return P.add("dve", lambda e, o=out, i=in_: e.reciprocal(out=o, in_=i), reads, writes)

    def ao_view(src3):
        return src3.rearrange("(k h) d t -> (h d) k t", h=2)

    def phase_outproj(name, ao_loader, x_loader, w_out_d, gain_d, x_dst, xdB, hT_dst, hTdB, router=False):
        ar.reset()
        wo, woB = ar.alloc([8, D], BF16)
        for kc in range(8):
            dma("pool", wo[:, kc, :], w_out_d[kc * 128:(kc + 1) * 128, :], writes=[woB])
        load_gain(name, gain_d)
        aoT = [ar.alloc([8, 512], BF16) for _ in range(2)]
        xts = [ar.alloc([4, D], F32) for _ in range(2)]
        hbs = [ar.alloc([D], BF16) for _ in range(2)]
        hTs = [ar.alloc([8, 512], BF16) for _ in range(2)]
        tmp = (ar.alloc([D], F32), ar.alloc([1], F32), ar.alloc([1], F32))
        if router:
            wr_t, wrB = ar.alloc([8, NEXP], F32)
            dma("sync", wr_t, w_r.rearrange("(k p) e -> p k e", p=128), writes=[wrB])
            hfs = [ar.alloc([D], F32) for _ in range(2)]
            hfT, hfTB = ar.alloc([8, 128], F32)
            r = {k: ar.alloc([8], F32) for k in ("lg", "mk1", "l2")}
            memset("dve", spm, 0.0, [spmB])
            r1 = {k: ar.alloc([1], F32) for k in ("m1", "m2", "dd", "ee", "dn", "g1", "g2")}

        def loads(ti):
            ao_loader(tiles[ti], aoT[ti % 2])
            x_loader(tiles[ti], xts[ti % 2])

        loads(0)
        for ti, (kind, sq, t0, TW, g0) in enumerate(tiles):
            if ti + 1 < len(tiles):
                loads(ti + 1)
            at, atB = aoT[ti % 2]
            xt, xB = xts[ti % 2]
            hT, hTB = hTs[ti % 2]
            nsub = TW // 128
            def stage1(s, ti=ti, g0=g0, at=at, atB=atB, xt=xt, xB=xB, hT=hT, hTB=hTB):
                for half in range(2):
                    pbi = (2 * s + half) % 4
                    for kc in range(8):
                        mm(pbanks[pbi][:, :], at[:, kc, s * 128:(s + 1) * 128], wo[:, kc, half * 512:(half + 1) * 512],
                           kc == 0, kc == 7, [atB, woB], [PB[pbi]])
                    tt("dve", xt[:, s, half * 512:(half + 1) * 512], xt[:, s, half * 512:(half + 1) * 512], pbanks[pbi][:, :],
                       ALU.add, [xB, PB[pbi]], [xB])
                hb, hbB = hbs[s % 2]
                rmsnorm_to_bf16(xt[:, s, :], xB, gtile[name], hb, hbB, 128, tmp)
                if not router:
                    transpose_to(hb, hbB, hT, hTB, s * 128, 128, 7)
                if router:
                    (rs, rsB) = tmp[2]
                    g = gtile[name]
                    hf, hfB = hfs[s % 2]
                    stt(hf, xt[:, s, :], rs[:, 0:1], g[0], ALU.mult, ALU.mult, [xB, rsB, g[1]], [hfB])
                    dma("sync", h4k[g0 + s * 128:g0 + (s + 1) * 128, :], hb, reads=[hbB], writes=[B_h4k])
            def stage2(s, g0=g0):
                hf, hfB = hfs[s % 2]
                for kc in range(8):
                    pbi = 4 + kc // 4
                    tr(pbanks[pbi][:, (kc % 4) * 128:(kc % 4 + 1) * 128], hf[:, kc * 128:(kc + 1) * 128], cf[:, C_ID, :],
                       [hfB, B_cf], [PB[pbi]])
                cp("act", hfT[:, 0:4, :], pbanks[4][:, :].rearrange("p (k t) -> p k t", k=4), [PB[4]], [hfTB])
                cp("act", hfT[:, 4:8, :], pbanks[5][:, :].rearrange("p (k t) -> p k t", k=4), [PB[5]], [hfTB])
                for kc in range(8):
                    mm(pbanks[6][:, 0:8], hfT[:, kc, :], wr_t[:, kc, :], kc == 0, kc == 7, [hfTB, wrB], [PB[6]])
                st_ = g0 // 128 + s
                (lg, lgB), (l2, l2B), (msum, msumB) = r["lg"], r["l2"], r["mk1"]
                (m1, m1B), (m2, m2B), (dd, ddB), (ee, eeB) = r1["m1"], r1["m2"], r1["dd"], r1["ee"]
                (dn, dnB) = r1["dn"]
                mk1, mk2 = MK1[:, st_, :], MK2[:, st_, :]
                cp("dve", lg, pbanks[6][:, 0:8], [PB[6]], [lgB])
                reduce_max(m1, lg, [lgB], [m1B])
                ts("dve", mk1, lg, m1[:, 0:1], ALU.is_equal, [lgB, m1B], [B_MK1])
                stt(l2, mk1, -1e30, lg, ALU.mult, ALU.add, [B_MK1, lgB], [l2B])
                reduce_max(m2, l2, [l2B], [m2B])
                ts("dve", mk2, l2, m2[:, 0:1], ALU.is_equal, [l2B, m2B], [B_MK2])
                tt("dve", dd, m2, m1, ALU.subtract, [m1B, m2B], [ddB])
                act(ee, dd, AF.Exp, [ddB], [eeB])
                ts("dve", dn, ee, 1.0, ALU.add, [eeB], [dnB])
                recip(G12[:, st_, 0:1], dn, [dnB], [B_G12])
                tt("dve", G12[:, st_, 1:2], ee, G12[:, st_, 0:1], ALU.mult, [eeB, B_G12], [B_G12])
                tt("dve", msum, mk1, mk2, ALU.add, [B_MK1, B_MK2], [msumB])
                mm(pbanks[6][:, 8:16], cf[:, C_TRS, :], msum, True, False, [B_cf, msumB], [PB[6]])
                mm(pbanks[6][:, 8:16], cf[:, C_ONE, :], spm, False, True, [B_cf, spmB], [PB[6]])
                cp("dve", RANK[:, st_, :], pbanks[6][:, 8:16], [PB[6]], [B_RK])
                tt("dve", spm, spm, msum, ALU.add, [spmB, msumB], [spmB])
            for s in range(nsub + 1):
                if s < nsub:
                    stage1(s)
                if router and s >= 1:
                    stage2(s - 1)
            dma("sync", x_dst[g0:g0 + TW, :].rearrange("(s p) d -> p s d", p=128), xt[:, 0:nsub, :], reads=[xB], writes=[xdB])
            if not router:
                dma("sync", hT_dst[:, :, g0:g0 + TW].rearrange("k p t -> p k t"), hT[:, :, 0:TW], reads=[hTB], writes=[hTdB])
        P.barrier()

    def ao_loader0(tile, buf):
        kind, sq, t0, TW, g0 = tile
        t, b = buf
        if kind == "p":
            dma("sync", t[:, :, 0:TW], ao_view(ao_p[sq])[:, :, t0:t0 + TW], reads=[B_ao_p], writes=[b])
        else:
            for s2 in range(2):
                dma("sync", t[:, :, s2 * 64:(s2 + 1) * 64], ao_view(ao_s[s2]), reads=[B_ao_s], writes=[b])

    def x_loader0(tile, buf):
        kind, sq, t0, TW, g0 = tile
        t, b = buf
        if kind == "p":
            dma("sync", t[:, 0:TW // 128, :], xp[sq, t0:t0 + TW, :].rearrange("(s p) d -> p s d", p=128), writes=[b])
        else:
            dma("sync", t[0:64, 0, :], xs[0], writes=[b])
            dma("sync", t[64:128, 0, :], xs[1], writes=[b])

    phase_outproj("ffn", ao_loader0, x_loader0, w_out_ab, g_ffn, x1, B_x1, h2t, B_h2t)
    if stop_after == 'C1':
        return nc, P, st, locals()

    def gate_up(hT, hTB, TW, wg_t, wgB, wu_t, wuB, nm, aT, aTB, sgs):
        for m in range(nm):
            pg, pu = (0, 1) if m % 2 == 0 else (2, 3)
            for kc in range(8):
                mm(pbanks[pg][:, 0:TW], wg_t[:, kc, m * 128:(m + 1) * 128], hT[:, kc, 0:TW], kc == 0, kc == 7, [wgB, hTB], [PB[pg]])
            for kc in range(8):
                mm(pbanks[pu][:, 0:TW], wu_t[:, kc, m * 128:(m + 1) * 128], hT[:, kc, 0:TW], kc == 0, kc == 7, [wuB, hTB], [PB[pu]])
            sg, sgB = sgs[m % 2]
            act(sg[:, 0:TW], pbanks[pg][:, 0:TW], AF.Silu, [PB[pg]], [sgB])
            tt("dve", aT[:, m, 0:TW], sg[:, 0:TW], pbanks[pu][:, 0:TW], ALU.mult, [sgB, PB[pu]], [aTB])

    ar.reset()
    wg_t, wgB = ar.alloc([8, D_FF], BF16)
    wu_t, wuB = ar.alloc([8, D_FF], BF16)
    for kc in range(8):
        dma("pool", wg_t[:, kc, :], w_g[kc * 128:(kc + 1) * 128, :], writes=[wgB])
        dma("pool", wu_t[:, kc, :], w_u[kc * 128:(kc + 1) * 128, :], writes=[wuB])
    hTs = [ar.alloc([8, 512], BF16) for _ in range(2)]
    aTs = [ar.alloc([22, 512], BF16) for _ in range(2)]
    sgs = [ar.alloc([512], F32) for _ in range(2)]

    def ld_c2(ti):
        kind, sq, t0, TW, g0 = tiles[ti]
        dma("sync", hTs[ti % 2][0][:, :, 0:TW], h2t[:, :, g0:g0 + TW].rearrange("k p t -> p k t"), reads=[B_h2t], writes=[hTs[ti % 2][1]])

    ld_c2(0)
    for ti, (kind, sq, t0, TW, g0) in enumerate(tiles):
        if ti + 1 < len(tiles):
            ld_c2(ti + 1)
        hT, hTB = hTs[ti % 2]
        aT, aTB = aTs[ti % 2]
        gate_up(hT, hTB, TW, wg_t, wgB, wu_t, wuB, 22, aT, aTB, sgs)
        dma("sync", actt[:, :, g0:g0 + TW].rearrange("m p t -> p m t"), aT[:, :, 0:TW], reads=[aTB], writes=[B_actt])
    P.barrier()
    if stop_after == 'C2':
        return nc, P, st, locals()

    ar.reset()
    wd_t, wdB = ar.alloc([22, D], BF16)
    for m in range(22):
        dma("pool", wd_t[:, m, :], w_d[m * 128:(m + 1) * 128, :], writes=[wdB])
    winc, wincB = ar.alloc([8, MIX_C_IN], BF16)
    for kc in range(8):
        dma("pool", winc[:, kc, :], w_in_c[kc * 128:(kc + 1) * 128, :], writes=[wincB])
    load_gain("c", g_c)
    aTs = [ar.alloc([22, 512], BF16) for _ in range(2)]
    xts = [ar.alloc([4, D], F32) for _ in range(2)]
    hbs = [ar.alloc([D], BF16) for _ in range(2)]
    hTs = [ar.alloc([8, 512], BF16) for _ in range(2)]
    tmp = (ar.alloc([D], F32), ar.alloc([1], F32), ar.alloc([1], F32))
    qsts = [ar.alloc([512], BF16) for _ in range(2)]
    kvts = [ar.alloc([256], F32) for _ in range(2)]
    vbts = [ar.alloc([128], BF16) for _ in range(2)]
    cws = [ar.alloc([128], F32) for _ in range(2)]

    def ld_c3(ti):
        kind, sq, t0, TW, g0 = tiles[ti]
        dma("sync", aTs[ti % 2][0][:, :, 0:TW], actt[:, :, g0:g0 + TW].rearrange("m p t -> p m t"), reads=[B_actt], writes=[aTs[ti % 2][1]])
        dma("sync", xts[ti % 2][0][:, 0:TW // 128, :], x1[g0:g0 + TW, :].rearrange("(s p) d -> p s d", p=128), reads=[B_x1],
            writes=[xts[ti % 2][1]])

    ld_c3(0)
    qi = 0
    for ti, (kind, sq, t0, TW, g0) in enumerate(tiles):
        if ti + 1 < len(tiles):
            ld_c3(ti + 1)
        aT, aTB = aTs[ti % 2]
        xt, xB = xts[ti % 2]
        hT, hTB = hTs[ti % 2]
        nsub = TW // 128
        for s in range(nsub):
            for half in range(2):
                pbi = (2 * s + half) % 4
                for m in range(22):
                    mm(pbanks[pbi][:, :], aT[:, m, s * 128:(s + 1) * 128], wd_t[:, m, half * 512:(half + 1) * 512],
                       m == 0, m == 21, [aTB, wdB], [PB[pbi]])
                tt("dve", xt[:, s, half * 512:(half + 1) * 512], xt[:, s, half * 512:(half + 1) * 512], pbanks[pbi][:, :],
                   ALU.add, [xB, PB[pbi]], [xB])
            hb, hbB = hbs[s % 2]
            rmsnorm_to_bf16(xt[:, s, :], xB, gtile["c"], hb, hbB, 128, tmp)
            transpose_to(hb, hbB, hT, hTB, s * 128, 128, 7)
        dma("sync", x2[g0:g0 + TW, :].rearrange("(s p) d -> p s d", p=128), xt[:, 0:nsub, :], reads=[xB], writes=[B_x2])
        for j in range(9):
            pbi = 4 + j % 2
            c0 = j * 128
            for kc in range(8):
                mm(pbanks[pbi][:, 0:TW], winc[:, kc, c0:c0 + 128], hT[:, kc, 0:TW], kc == 0, kc == 7, [wincB, hTB], [PB[pbi]])
            qs, qsB = qsts[qi % 2]; qi += 1
            if j < 8:
                act(qs[:, 0:TW], pbanks[pbi][:, 0:TW], AF.Copy, [PB[pbi]], [qsB], scale=0.125)
                dma("sync", qct[2 * j:2 * j + 2, :, g0:g0 + TW].rearrange("h d t -> (h d) t"), qs[:, 0:TW], reads=[qsB], writes=[B_qct])
            else:
                cp("dve", qs[:, 0:TW], pbanks[pbi][:, 0:TW], [PB[pbi]], [qsB])
                dma("sync", kct[0:2, :, g0:g0 + TW].rearrange("h d t -> (h d) t"), qs[:, 0:TW], reads=[qsB], writes=[B_kct])
        for s in range(nsub):
            pbi = 6
            for kc in range(8):
                mm(pbanks[pbi][:, 0:256], hT[:, kc, s * 128:(s + 1) * 128], winc[:, kc, 1024:1280], kc == 0, kc == 7,
                   [wincB, hTB], [PB[pbi]])
            kvt, kvB = kvts[s % 2]
            vbt, vbB = vbts[s % 2]
            cp("act", kvt, pbanks[pbi][:, 0:256], [PB[pbi]], [kvB])
            cp("dve", vbt, pbanks[pbi][:, 128:256], [PB[pbi]], [vbB])
            dma("sync", vc[g0 + s * 128:g0 + (s + 1) * 128, :], vbt, reads=[vbB], writes=[B_vc])
            if kind == "p":
                if t0 + s * 128 == SEQ - 128:
                    dma("sync", o_pwk[sq], kvt[:, 0:128], reads=[kvB], writes=[OUTB])
                    dma("sync", o_pwv[sq], kvt[:, 128:256], reads=[kvB], writes=[OUTB])
            else:
                for s2 in range(2):
                    dma("sync", o_swk[s2, 64:128, :], kvt[s2 * 64:(s2 + 1) * 64, 0:128], reads=[kvB], writes=[OUTB])
                    dma("sync", o_swv[s2, 64:128, :], kvt[s2 * 64:(s2 + 1) * 64, 128:256], reads=[kvB], writes=[OUTB])
                    for ci, (csrc, odst) in enumerate(((c_wk, o_swk), (c_wv, o_swv))):
                        cw, cwB = cws[ci]
                        dma("sync", cw[0:64, :], csrc[s2, 64:128, :], writes=[cwB])
                        dma("sync", odst[s2, 0:64, :], cw[0:64, :], reads=[cwB], writes=[OUTB])
    P.barrier()
    if stop_after == 'C3':
        return nc, P, st, locals()

    ar.reset()
    alib, alibB = ar.alloc([16, 256], F32)
    dma("sync", alib, alibi, writes=[alibB])
    esk, eskB = ar.alloc([16], F32)
    dma("sync", esk, sinks[0:1, :].partition_broadcast(128).rearrange("p o d -> p (o d)"), writes=[eskB])
    act(esk, esk, AF.Exp, [eskB], [eskB])
    kTq = [ar.alloc([2, SEQ], BF16, parts=64) for _ in range(1)]
    vsq = [ar.alloc([SEQ // 128, 2, 128], BF16) for _ in range(1)]
    memset("pool", vsq[0][0][:, :, :, 64:128], 1.0, [vsq[0][1]])
    alb, albB = ar.alloc([2, 16, 256], BF16)
    for hl in range(2):
        for hh_ in range(4):
            dma("pool", alb[:, hl, 4 * hh_:4 * hh_ + 4, :], alibi2[:, hl, 4 * hh_:4 * hh_ + 4, :], writes=[albB])
    qTt = [ar.alloc([16, 512], BF16, parts=64) for _ in range(2)]
    sA = [ar.alloc([512], F32) for _ in range(2)]
    pA_ = [ar.alloc([512], BF16) for _ in range(2)]
    sB = [ar.alloc([512], F32) for _ in range(2)]
    pB_ = [ar.alloc([512], BF16) for _ in range(2)]
    dn_t, dnB_ = ar.alloc([512], F32)
    dns = [ar.alloc([512], F32) for _ in range(2)]
    aos = [ar.alloc([512], BF16) for _ in range(2)]
    ckf = [ar.alloc([128], BF16) for _ in range(2)]
    kTc, kTcB = ar.alloc([2, 128], BF16, parts=64)
    kTn, kTnB = ar.alloc([2, 64], BF16, parts=64)
    vn, vnB = ar.alloc([128], BF16, parts=64)
    qTs_, qTsB = ar.alloc([16, 64], BF16, parts=64)
    ui = 0
    for sq in range(2):
        (kT, kTB), (vs, vsB) = kTq[0], vsq[0]
        g0s = sq * SEQ
        dma("sync", kT[0:64, :, :], kct[:, :, g0s:g0s + SEQ].rearrange("h d t -> d h t"), reads=[B_kct], writes=[kTB])
        for kvh_ in range(2):
            dma("sync", vs[:, :, kvh_, 0:64], vc[g0s:g0s + SEQ, kvh_ * 64:(kvh_ + 1) * 64].rearrange("(k p) f -> p k f", p=128),
                reads=[B_vc], writes=[vsB])
        for qg in range(SEQ // 512):
            qT, qTB = qTt[qg % 2]
            dma("sync", qT[0:64, :, :], qct[:, :, g0s + qg * 512:g0s + (qg + 1) * 512].rearrange("h d t -> d h t"),
                reads=[B_qct], writes=[qTB])
            units = [(qq, grp) for qq in range(4) for grp in range(4)]

            def swa_s1(u, ui_):
                qq, grp = units[u]
                qb = qg * 4 + qq
                kvh = grp // 2
                a = ui_ % 2
                bA, bB, bO = (0, 1, 2) if a == 0 else (4, 5, 6)
                rhs = qT[0:64, 4 * grp:4 * grp + 4, qq * 128:(qq + 1) * 128]
                for (bk, kb_, c0_, pt) in ((bA, qb, 0, pA_[a]), (bB, qb - 1, 128, pB_[a])):
                    if kb_ < 0:
                        continue
                    mm(pbanks[bk][:, :], kT[0:64, kvh, kb_ * 128:(kb_ + 1) * 128], rhs, True, False, [kTB, qTB], [PB[bk]])
                    mm(pbanks[bk][:, :], cb[:, C_ID, :], alb[:, 0, 4 * grp:4 * grp + 4, c0_:c0_ + 128], False, False, [B_cb, albB], [PB[bk]])
                    mm(pbanks[bk][:, :], cb[:, C_ID, :], alb[:, 1, 4 * grp:4 * grp + 4, c0_:c0_ + 128], False, True, [B_cb, albB], [PB[bk]])
                    act(pt[0], pbanks[bk][:, :], AF.Exp, [PB[bk]], [pt[1]])

            def swa_s2(u, ui_):
                qq, grp = units[u]
                qb = qg * 4 + qq
                kvh = grp // 2
                a = ui_ % 2
                bA, bB, bO = (0, 1, 2) if a == 0 else (4, 5, 6)
                (p_a, p_aB), (p_b, p_bB) = pA_[a], pB_[a]
                mm(pbanks[bO][:, :], vs[:, qb, kvh, :], p_a, True, qb == 0, [vsB, p_aB], [PB[bO]])
                if qb > 0:
                    mm(pbanks[bO][:, :], vs[:, qb - 1, kvh, :], p_b, False, True, [vsB, p_bB], [PB[bO]])
                dn_, dnB2 = dns[a]
                for hh in range(4):
                    act(dn_[64:128, hh * 128:(hh + 1) * 128], pbanks[bO][64:128, hh * 128:(hh + 1) * 128], AF.Ln,
                        [PB[bO], eskB], [dnB2], bias=esk[64:128, 4 * grp + hh:4 * grp + hh + 1])
                ao, aoB = aos[a]
                act(dn_[64:128, :], dn_[64:128, :], AF.Exp, [dnB2], [dnB2], scale=-1.0)
                tt("dve", ao[0:64, :], pbanks[bO][0:64, :], dn_[64:128, :], ALU.mult, [PB[bO], dnB2], [aoB])
                dma("sync", ao_c[4 * grp:4 * grp + 4, :, g0s + qb * 128:g0s + (qb + 1) * 128].rearrange("h d t -> d h t"),
                    ao[0:64, :].rearrange("p (h t) -> p h t", h=4), reads=[aoB], writes=[B_aoc])

            for u in range(len(units) + 1):
                if u < len(units):
                    swa_s1(u, ui + u)
                if u >= 1:
                    swa_s2(u - 1, ui + u - 1)
            ui += len(units)
    for s2 in range(2):
        g0s = 2 * SEQ + s2 * 64
        (ck, ckB), (cv, cvB) = ckf[0], ckf[1]
        dma("pool", ck, c_wk[s2], writes=[ckB])
        dma("pool", cv, c_wv[s2], writes=[cvB])
        ptb = pbanks[7][:].bitcast(BF16)
        for kvh in range(2):
            tr(ptb[0:64, kvh * 128:(kvh + 1) * 128], ck[:, kvh * 64:(kvh + 1) * 64], cb[:, C_ID, :], [ckB, B_cb], [PB[7]])
        cp("act", kTc[0:64, :, :], ptb[0:64, 0:256].rearrange("p (h t) -> p h t", h=2), [PB[7]], [kTcB])
        dma("sync", kTn[0:64, :, :], kct[:, :, g0s:g0s + 64].rearrange("h d t -> d h t"), reads=[B_kct], writes=[kTnB])
        dma("sync", vn[0:64, :], vc[g0s:g0s + 64, :], reads=[B_vc], writes=[vnB])
        dma("sync", qTs_[0:64, :, :], qct[:, :, g0s:g0s + 64].rearrange("h d t -> d h t"), reads=[B_qct], writes=[qTsB])
        for grp in range(4):
            kvh = grp // 2
            a = ui % 2; ui += 1
            bA, bB, bO, bD = (0, 1, 2, 3) if a == 0 else (4, 5, 6, 7)
            rhs = qTs_[0:64, 4 * grp:4 * grp + 4, :]
            (s_a, s_aB), (p_a, p_aB) = sA[a], pA_[a]
            (s_b, s_bB), (p_b, p_bB) = sB[a], pB_[a]
            mm(pbanks[bA][0:64, 0:256], kTn[0:64, kvh, :], rhs, True, True, [kTnB, qTsB], [PB[bA]])
            tt("dve", s_a[0:64, 0:256].rearrange("p (h t) -> p h t", h=4), pbanks[bA][0:64, 0:256].rearrange("p (h t) -> p h t", h=4),
               alib[0:64, 4 * grp:4 * grp + 4, 0:64], ALU.add, [PB[bA], alibB], [s_aB])
            act(p_a[0:64, 0:256], s_a[0:64, 0:256], AF.Exp, [s_aB], [p_aB])
            mm(pbanks[bB][:, 0:256], kTc[0:64, kvh, :], rhs, True, True, [kTcB, qTsB], [PB[bB]])
            tt("dve", s_b[:, 0:256].rearrange("p (h t) -> p h t", h=4), pbanks[bB][:, 0:256].rearrange("p (h t) -> p h t", h=4),
               alib[:, 4 * grp:4 * grp + 4, 128:192], ALU.add, [PB[bB], alibB], [s_bB])
            act(p_b[:, 0:256], s_b[:, 0:256], AF.Exp, [s_bB], [p_bB])
            mm(pbanks[bO][0:64, 0:256], vn[0:64, kvh * 64:(kvh + 1) * 64], p_a[0:64, 0:256], True, False, [vnB, p_aB], [PB[bO]])
            mm(pbanks[bO][0:64, 0:256], cv[:, kvh * 64:(kvh + 1) * 64], p_b[:, 0:256], False, True, [cvB, p_bB], [PB[bO]])
            mm(pbanks[bD][0:64, 0:256], cb[0:64, C_ONE, 0:64], p_a[0:64, 0:256], True, False, [B_cb, p_aB], [PB[bD]])
            mm(pbanks[bD][0:64, 0:256], cb[:, C_ONE, 0:64], p_b[:, 0:256], False, True, [B_cb, p_bB], [PB[bD]])
            for hh in range(4):
                ts("dve", dn_t[0:64, hh * 64:(hh + 1) * 64], pbanks[bD][0:64, hh * 64:(hh + 1) * 64],
                   esk[0:64, 4 * grp + hh:4 * grp + hh + 1], ALU.add, [PB[bD], eskB], [dnB_])
            recip(dn_t[0:64, 0:256], dn_t[0:64, 0:256], [dnB_], [dnB_])
            ao, aoB = aos[a]
            tt("dve", ao[0:64, 0:256], pbanks[bO][0:64, 0:256], dn_t[0:64, 0:256], ALU.mult, [PB[bO], dnB_], [aoB])
            dma("sync", ao_c[4 * grp:4 * grp + 4, :, g0s:g0s + 64].rearrange("h d t -> d h t"),
                ao[0:64, 0:256].rearrange("p (h t) -> p h t", h=4), reads=[aoB], writes=[B_aoc])
    P.barrier()
    if stop_after == 'D2':
        return nc, P, st, locals()

    def ao_loader1(tile, buf):
        kind, sq, t0, TW, g0 = tile
        t, b = buf
        dma("sync", t[:, :, 0:TW], ao_view(ao_c)[:, :, g0:g0 + TW], reads=[B_aoc], writes=[b])

    def x_loader1(tile, buf):
        kind, sq, t0, TW, g0 = tile
        t, b = buf
        dma("sync", t[:, 0:TW // 128, :], x2[g0:g0 + TW, :].rearrange("(s p) d -> p s d", p=128), reads=[B_x2], writes=[b])

    phase_outproj("moe", ao_loader1, x_loader1, w_out_c, g_moe, x3, B_x3, h4t, B_h4t, router=True)
    if stop_after == 'D3':
        return nc, P, st, locals()

    B_ind = Buf("indirect_chain")
    import os as _os2
    CHAIN = _os2.environ.get("CHAIN", "1") == "1"

    def idma_gather(out, table, idx_ap, reads, writes):
        if CHAIN:
            reads = list(reads) + [B_ind]
            writes = list(writes) + [B_ind]
        return P.add("pool", lambda e, o=out, t=table, ix=idx_ap: e.indirect_dma_start(
            out=o, out_offset=None, in_=t, in_offset=bass.IndirectOffsetOnAxis(ap=ix, axis=0)), reads, writes, is_dma=True)

    def idma_scatter(table, idx_ap, in_, reads, writes):
        if CHAIN:
            reads = list(reads) + [B_ind]
            writes = list(writes) + [B_ind]
        return P.add("pool", lambda e, o=table, s_=in_, ix=idx_ap: e.indirect_dma_start(
            out=o, out_offset=bass.IndirectOffsetOnAxis(ap=ix, axis=0), in_=s_, in_offset=None), reads, writes, is_dma=True)

    ar.reset()
    c8 = {k: ar.alloc([NEXP], F32) for k in ("t", "m", "pad", "pend", "pst")}
    mm(pbanks[6][:, 16:24], cf[:, C_ONE, :], spm, True, True, [B_cf, spmB], [PB[6]])
    cp("dve", c8["t"][0], pbanks[6][:, 16:24], [PB[6]], [c8["t"][1]])
    t48, t48B = ar.alloc([48], F32)
    for e in range(NEXP):
        ts("dve", t48, iota_t[:, 8:56], c8["t"][0][:, e:e + 1], ALU.subtract, [B_iota, c8["t"][1]], [t48B], s2=-1.0, op1=ALU.mult)
        ts("dve", t48, t48, 0.0, ALU.max, [t48B], [t48B], s2=1.0, op1=ALU.min)
        P.add("dve", lambda e_, o=c8["m"][0][:, e:e + 1], i=t48: e_.tensor_reduce(out=o, in_=i, axis=AX, op=ALU.add), [t48B], [c8["m"][1]])
    ts("dve", c8["pad"][0], c8["m"][0], 512.0, ALU.mult, [c8["m"][1]], [c8["pad"][1]])
    pend, pendB = c8["pend"]
    pad_, padB = c8["pad"]
    cp("dve", pend[:, 0:1], pad_[:, 0:1], [padB], [pendB])
    for e in range(1, NEXP):
        tt("dve", pend[:, e:e + 1], pend[:, e - 1:e], pad_[:, e:e + 1], ALU.add, [pendB, padB], [pendB])
    pst, pstB = c8["pst"]
    tt("dve", pst, pend, pad_, ALU.subtract, [pendB, padB], [pstB])
    for st_ in range(NT):
        tt("dve", RANK[:, st_, :], RANK[:, st_, :], pst, ALU.add, [B_RK, pstB], [B_RK])
    TMP, TMPB = ar.alloc([NT, NEXP], F32)
    slf = [ar.alloc([NT], F32) for _ in range(2)]
    for k, (MK, B_MK) in enumerate(((MK1, B_MK1), (MK2, B_MK2))):
        tt("dve", TMP, MK, RANK, ALU.mult, [B_MK, B_RK], [TMPB])
        P.add("dve", lambda e, o=slf[k][0], i=TMP: e.tensor_reduce(out=o, in_=i, axis=AX, op=ALU.add), [TMPB], [slf[k][1]])
        cp("dve", SL[:, :, k], slf[k][0], [slf[k][1]], [B_SL])
    eb, ebB = ar.alloc([48], F32)
    memset("dve", eb, 0.0, [ebB])
    for e in range(NEXP):
        ts("dve", t48, iota_t[:, 8:56], pend[:, e:e + 1], ALU.subtract, [B_iota, pendB], [t48B], s2=1.0, op1=ALU.add)
        ts("dve", t48, t48, 0.0, ALU.max, [t48B], [t48B], s2=1.0, op1=ALU.min)
        tt("dve", eb, eb, t48, ALU.add, [ebB, t48B], [ebB])
    ts("dve", eb, eb, float(NEXP - 1), ALU.min, [ebB], [ebB])
    IDXf, IDXfB = ar.alloc([NBLK, 4], F32)
    IDX, B_IDX = ar.alloc([NBLK, 4], I32)
    for q in range(4):
        ts("dve", IDXf[:, :, q], eb[:, 0:NBLK], 512.0, ALU.mult, [ebB], [IDXfB], s2=q * 128.0, op1=ALU.add)
        ts("dve", IDXf[:, :, q], IDXf[:, :, q], iota_t[:, 56:57], ALU.add, [IDXfB, B_iota], [IDXfB])
    cp("dve", IDX, IDXf, [IDXfB], [B_IDX])
    if stop_after == 'E0':
        dbg = y_s.rearrange("a q d -> (a q) d")
        dma("sync", dbg[:, 0:NT], slf[0][0], reads=[slf[0][1]], writes=[OUTB])
        dma("sync", dbg[:, 16:16 + NT], slf[1][0], reads=[slf[1][1]], writes=[OUTB])
        dma("sync", dbg[:, 32:80], eb, reads=[ebB], writes=[OUTB])
        dma("sync", dbg[:, 80:88], pend, reads=[pendB], writes=[OUTB])
        dma("sync", dbg[:, 88:96], c8["t"][0], reads=[c8["t"][1]], writes=[OUTB])
        dma("sync", dbg[:, 100:100 + NBLK * 4], IDXf.rearrange("p b q -> p (b q)"), reads=[IDXfB], writes=[OUTB])
        dma("sync", dbg[:, 200:200 + NT * 2], SL.rearrange("p a b -> p (a b)").bitcast(F32), reads=[B_SL], writes=[OUTB])
        dma("sync", dbg[:, 300:300 + NBLK * 4], IDX.rearrange("p a b -> p (a b)").bitcast(F32), reads=[B_IDX], writes=[OUTB])
        P.barrier()
        return nc, P, st, locals()
    xks = [ar.alloc([4, D], BF16) for _ in range(2)]
    z8, z8B = xks[0]
    memset("dve", z8, 0.0, [z8B])
    for b_ in range(NBLK):
        dma("sync", xsl[b_ * 512:(b_ + 1) * 512, :].rearrange("(s p) d -> p s d", p=128), z8, reads=[z8B], writes=[B_xsl])
    hks = [ar.alloc([D], BF16) for _ in range(2)]
    for st_ in range(NT):
        hk, hkB = hks[st_ % 2]
        dma("sync", hk, h4k[st_ * 128:(st_ + 1) * 128, :], reads=[B_h4k], writes=[hkB])
        for k in range(2):
            idma_scatter(xsl[:, :], SL[:, st_, k:k + 1], hk, [B_SL, hkB, B_xsl], [B_xsl])
    if stop_after == 'E1':
        P.barrier()
        return nc, P, st, locals()
    wq = [(ar.alloc([8, 896], BF16), ar.alloc([8, 896], BF16), ar.alloc([7, D], BF16)) for _ in range(2)]
    xTs = [ar.alloc([8, 512], BF16) for _ in range(2)]
    aTs = [ar.alloc([7, 512], BF16) for _ in range(2)]
    sgs = [ar.alloc([512], F32) for _ in range(2)]
    yss = [ar.alloc([4, D], F32) for _ in range(2)]

    def ld_blk(b_):
        dma("sync", xks[b_ % 2][0], xsl[b_ * 512:(b_ + 1) * 512, :].rearrange("(s p) d -> p s d", p=128), reads=[B_xsl], writes=[xks[b_ % 2][1]])

    def ld_w(j):
        b_, q = j // 4, j % 4
        (wg, wgB), (wu, wuB), (wd, wdB) = wq[j % 2]
        idma_gather(wg.rearrange("p k m -> p (k m)"), wge_s[:, :], IDX[:, b_, q:q + 1], [B_IDX, B_wge], [wgB])
        idma_gather(wu.rearrange("p k m -> p (k m)"), wue_s[:, :], IDX[:, b_, q:q + 1], [B_IDX, B_wue], [wuB])
        idma_gather(wd.rearrange("p k m -> p (k m)"), wde_s[:, :], IDX[:, b_, q:q + 1], [B_IDX, B_wde], [wdB])

    ld_blk(0)
    ld_w(0)
    for b_ in range(NBLK):
        if b_ + 1 < NBLK:
            ld_blk(b_ + 1)
        xk, xkB = xks[b_ % 2]
        xT, xTB = xTs[b_ % 2]
        ys, ysB = yss[b_ % 2]
        for s in range(4):
            transpose_to(xk[:, s, :], xkB, xT, xTB, s * 128, 128, 7)
        for q in range(4):
            j = b_ * 4 + q
            if j + 1 < NBLK * 4:
                ld_w(j + 1)
            (wg, wgB), (wu, wuB), (wd, wdB) = wq[j % 2]
            aT, aTB = aTs[j % 2]
            gate_up(xT, xTB, 512, wg, wgB, wu, wuB, 7, aT, aTB, sgs)
            for s in range(4):
                for half in range(2):
                    pbi = 4 + (2 * s + half) % 3
                    for m in range(7):
                        mm(pbanks[pbi][:, :], aT[:, m, s * 128:(s + 1) * 128], wd[:, m, half * 512:(half + 1) * 512],
                           m == 0, m == 6, [aTB, wdB], [PB[pbi]])
                    ysl_ = ys[:, s, half * 512:(half + 1) * 512]
                    if q == 0:
                        cp("dve", ysl_, pbanks[pbi][:, :], [PB[pbi]], [ysB])
                    else:
                        tt("dve", ysl_, pbanks[pbi][:, :], ysl_, ALU.add, [PB[pbi], ysB], [ysB])
        dma("sync", ysl[b_ * 512:(b_ + 1) * 512, :].rearrange("(s p) d -> p s d", p=128), ys, reads=[ysB], writes=[B_ysl])
    P.barrier()
    ar.reset()
    load_gain("fin", g_fin)
    x3s = [ar.alloc([D], F32) for _ in range(2)]
    y1s = [ar.alloc([D], F32) for _ in range(2)]
    y2s = [ar.alloc([D], F32) for _ in range(2)]
    yo = [ar.alloc([D], F32) for _ in range(2)]
    tmp = (ar.alloc([D], F32), ar.alloc([1], F32), ar.alloc([1], F32))
    subt = []
    for (kind, sq, t0, TW, g0) in tiles:
        for s in range(TW // 128):
            subt.append((kind, sq, t0 + s * 128))
    assert len(subt) == NT

    def ld_e3(st_):
        dma("sync", x3s[st_ % 2][0], x3[st_ * 128:(st_ + 1) * 128, :], reads=[B_x3], writes=[x3s[st_ % 2][1]])
        idma_gather(y1s[st_ % 2][0], ysl[:, :], SL[:, st_, 0:1], [B_SL, B_ysl], [y1s[st_ % 2][1]])
        idma_gather(y2s[st_ % 2][0], ysl[:, :], SL[:, st_, 1:2], [B_SL, B_ysl], [y2s[st_ % 2][1]])

    ld_e3(0)
    for st_, (kind, sq, r0) in enumerate(subt):
        if st_ + 1 < NT:
            ld_e3(st_ + 1)
        (xt, xB), (y1, y1B), (y2, y2B) = x3s[st_ % 2], y1s[st_ % 2], y2s[st_ % 2]
        stt(xt, y1, G12[:, st_, 0:1], xt, ALU.mult, ALU.add, [y1B, B_G12, xB], [xB])
        stt(xt, y2, G12[:, st_, 1:2], xt, ALU.mult, ALU.add, [y2B, B_G12, xB], [xB])
        (sq_, sqB), (ss, ssB), (rs, rsB) = tmp
        g = gtile["fin"]
        act(sq_, xt, AF.Square, [xB], [sqB, ssB], accum=ss)
        act(rs, ss, AF.Ln, [ssB], [rsB], scale=1.0 / D, bias=EPS)
        act(rs, rs, AF.Exp, [rsB], [rsB], scale=-0.5)
        yo_, yoB = yo[st_ % 2]
        stt(yo_, xt, rs[:, 0:1], g[0], ALU.mult, ALU.mult, [xB, rsB, g[1]], [yoB])
        if kind == "p":
            dma("sync", y_p[sq, r0:r0 + 128, :], yo_, reads=[yoB], writes=[OUTB])
        else:
            dma("sync", y_s[0], yo_[0:64, :], reads=[yoB], writes=[OUTB])
            dma("sync", y_s[1], yo_[64:128, :], reads=[yoB], writes=[OUTB])
    P.barrier()
    return nc, P, st, locals()


def make_consts():
    j = np.arange(128)[:, None]
    s = np.arange(128)[None, :]
    c = np.zeros((128, 14, 128), np.float32)
    c[:, 0] = (j == s)
    c[:, 1] = -(j >= s).astype(np.float32)
    c[:, 2] = (j < s)
    c[:, 3] = (j <= s)
    c[:, 4] = np.where(j < s, 0.0, NEGBIG)
    c[:, 5] = np.where(j <= s, 0.0, NEGBIG)
    c[:, 6] = (j <= s)
    c[:, 7] = 1.0
    c[:, 8] = (s < 64) * np.ones((128, 1))
    c[:, 9] = (s >= 64) * np.ones((128, 1))
    c[:, 10] = (j < s)
    c[:, 11] = 0.0
    c[:, 11, 64] = -1.0
    c[:, 12] = (j <= s) & ((j < 64) == (s < 64))
    c[:, 13] = (j == 0) * np.ones((1, 128))
    return c


def make_alibi():
    sk = np.arange(128)[:, None]
    tq = np.arange(256)[None, :]
    vis = ((tq // 64) - (sk // 64) >= 0) & ((tq // 64) - (sk // 64) <= 2)
    dist = np.abs(tq - sk).astype(np.float32)
    out = np.zeros((128, 16, 256), np.float32)
    for h in range(16):
        slope = np.float32(2.0 ** (-8.0 * (h + 1.0) / 16))
        out[:, h, :] = np.where(vis, -slope * dist, NEGBIG)
    return out


def make_iota():
    t = np.zeros((128, 64), np.float32)
    t[:, 0:8] = np.arange(8)[None, :] * 128 + np.arange(128)[:, None]
    t[:, 8:56] = np.arange(48)[None, :] * 512.0
    t[:, 56] = np.arange(128)
    return t


_CACHE = {}


def run(inputs, SEQ, PAST, ncores=8):
    key = (SEQ, PAST)
    if key not in _CACHE:
        import os
        nc, P, st, _ = build(SEQ, PAST, os.environ.get('STOP'))
        P.emit()
        _CACHE[key] = nc
    nc = _CACHE[key]
    f = lambda a: np.ascontiguousarray(np.asarray(a, dtype=np.float32))
    I = {k: np.asarray(v) for k, v in inputs.items()}
    _al = make_alibi()
    _hi = _al.astype(BF16NP).astype(np.float32)
    consts = {"cst": make_consts(), "alibi": _al, "alibi2": np.ascontiguousarray(np.stack([_hi, _al - _hi], axis=1)), "iot": make_iota()}
    shared = {
        "g_ab": f(I["norm_mix_ab"][0:1]), "w_in_ab": f(I["w_in_ab"][0]), "b_fg": f(I["b_forget"][0:1]),
        "w_out_ab": f(I["w_out_ab"][0]), "g_ffn": f(I["norm_ffn_dense"][0:1]),
        "w_g": f(I["w_gate_dense"][0]), "w_u": f(I["w_up_dense"][0]), "w_d": f(I["w_down_dense"][0]),
        "g_c": f(I["norm_mix_c"][0:1]), "w_in_c": f(I["w_in_c"][0]), "sinks": f(I["sinks"][0:1]),
        "w_out_c": f(I["w_out_c"][0]), "g_moe": f(I["norm_ffn_moe"][0:1]), "w_r": f(I["w_router"][0]),
        "w_ge": f(I["w_gate_moe"][0]), "w_ue": f(I["w_up_moe"][0]), "w_de": f(I["w_down_moe"][0]),
        "g_fin": f(I["norm_final"][None, :]),
    }
    shared.update(consts)
    in_maps = []
    for c in range(ncores):
        sl = slice(2 * c, 2 * c + 2)
        m = dict(shared)
        m["xp"] = f(I["x_prompt"][sl]); m["xs"] = f(I["x_sample"][sl])
        m["c_sbk"] = f(I["cache_sb_k"][0, sl]).reshape(2, PAST, 512)
        m["c_sbv"] = f(I["cache_sb_v"][0, sl]).reshape(2, PAST, 512)
        m["c_fxk"] = f(I["cache_fox_k"][0, sl]).reshape(2, PAST, 512)
        m["c_fxv"] = f(I["cache_fox_v"][0, sl]).reshape(2, PAST, 512)
        m["c_lf"] = f(I["cache_fox_logf"][0, sl])
        m["c_wk"] = f(I["cache_swa_k"][0, sl]).reshape(2, 128, 128)
        m["c_wv"] = f(I["cache_swa_v"][0, sl]).reshape(2, 128, 128)
        in_maps.append(m)
    res = run_bass_kernel_spmd(nc, in_maps, core_ids=list(range(ncores)))
    R = res.results
    cat = lambda k: np.concatenate([np.asarray(r[k]) for r in R], axis=0)
    B = 2 * ncores
    outs = (
        cat("y_p"), cat("y_s"),
        cat("p_sbk").reshape(1, B, SEQ, 8, 64), cat("p_sbv").reshape(1, B, SEQ, 8, 64),
        cat("p_fxk").reshape(1, B, SEQ, 8, 64), cat("p_fxv").reshape(1, B, SEQ, 8, 64),
        cat("p_lf").reshape(1, B, SEQ, 8),
        cat("p_wk").reshape(1, B, 128, 2, 64), cat("p_wv").reshape(1, B, 128, 2, 64),
        cat("s_sbk").reshape(1, B, NQ, 8, 64), cat("s_sbv").reshape(1, B, NQ, 8, 64),
        cat("s_fxk").reshape(1, B, NQ, 8, 64), cat("s_fxv").reshape(1, B, NQ, 8, 64),
        cat("s_lf").reshape(1, B, NQ, 8),
        cat("s_wk").reshape(1, B, 128, 2, 64), cat("s_wv").reshape(1, B, 128, 2, 64),
    )
    return tuple(np.ascontiguousarray(o, dtype=np.float32) for o in outs)


def kernel(**inputs):
    return run(inputs, 4096, 4096, 8)
```

```python
import contextlib
import numpy as np
import ml_dtypes
import concourse.bass as bass
import concourse.mybir as mybir
from concourse.bass_utils import run_bass_kernel_spmd

F32 = mybir.dt.float32
BF16NP = ml_dtypes.bfloat16
BF16 = mybir.dt.bfloat16
I32 = mybir.dt.int32
AF = mybir.ActivationFunctionType
ALU = mybir.AluOpType

D = 1024
NQ = 64
HD = 64
D_FF = 2816
D_EXP = 3584
NEXP = 8
MIX_AB_IN = 3080
MIX_C_IN = 1280
NEGBIG = -30000.0
EPS = 1e-6
COMPUTE = ("pe", "act", "dve", "pool")


class Op:
    __slots__ = ("eng", "fn", "deps", "is_dma", "seq", "ring", "ringval", "has_dependents")

    def __init__(self, eng, fn, is_dma=False):
        self.eng = eng
        self.fn = fn
        self.deps = []
        self.is_dma = is_dma
        self.has_dependents = False
        self.seq = None
        self.ring = None
        self.ringval = None


class Buf:
    __slots__ = ("name", "writers", "readers", "multi", "excl")

    def __init__(self, name="", multi=False, excl=False):
        self.name = name
        self.writers = []
        self.readers = []
        self.multi = multi
        self.excl = excl


class Prog:
    EP = 30000
    REP = 2000

    def __init__(self, nc, n_ring=8):
        self.nc = nc
        self.ops = {e: [] for e in ("pe", "act", "dve", "pool", "sync")}
        self.n_ring = n_ring
        self.n_ops = 0

    def add(self, eng, fn, reads=(), writes=(), is_dma=False):
        op = Op(eng, fn, is_dma)
        deps = set()
        for b in reads:
            deps.update(b.writers)
            if b.excl:
                deps.update(r for r in b.readers if r.eng != eng)
        for b in writes:
            if not b.multi:
                deps.update(b.writers)
                deps.update(b.readers)
        for b in reads:
            if not b.multi:
                b.readers.append(op)
        for b in writes:
            if b.multi:
                b.writers.append(op)
            else:
                b.writers = [op]
                b.readers = []
        deps.discard(op)
        for d in deps:
            if d.eng == "pe" and eng == "pe" and not d.is_dma and not is_dma:
                continue
            op.deps.append(d)
            d.has_dependents = True
        self.ops[eng].append(op)
        self.n_ops += 1
        return op

    def barrier(self):
        lasts = []
        for e in COMPUTE:
            for op in reversed(self.ops[e]):
                if not op.is_dma and op.fn is not None:
                    lasts.append(op)
                    break
        for q in ("sync", "pool"):
            k = 0
            for op in reversed(self.ops[q]):
                if op.is_dma:
                    lasts.append(op)
                    k += 1
                    if k >= self.n_ring:
                        break
        for e in self.ops:
            op = Op(e, None)
            for d in lasts:
                op.deps.append(d)
                d.has_dependents = True
            self.ops[e].append(op)

    def emit(self):
        nc = self.nc
        self.barrier()
        EP = self.EP
        REP = self.REP
        with contextlib.ExitStack() as st:
            nep = {}
            for e in COMPUTE:
                c = 0
                for op in self.ops[e]:
                    if op.is_dma or op.fn is None:
                        continue
                    if op.has_dependents:
                        op.seq = (c // EP, c % EP + 1)
                        c += 1
                nep[e] = max(1, (c + EP - 1) // EP)
            sems = {e: [st.enter_context(nc.semaphore(f"s_{e}_{i}")) for i in range(nep[e])] for e in COMPUTE}
            rings = {}
            for q in ("sync", "pool"):
                k = 0
                for op in self.ops[q]:
                    if op.is_dma:
                        use = k // self.n_ring
                        op.ring = (use // REP, k % self.n_ring)
                        op.ringval = (16 * (use % REP + 1), use)
                        k += 1
                nuse = (k + self.n_ring - 1) // self.n_ring
                nrep = max(1, (nuse + REP - 1) // REP)
                rings[q] = [[st.enter_context(nc.semaphore(f"r_{q}_{j}_{i}")) for i in range(self.n_ring)] for j in range(nrep)]
            block = st.enter_context(nc.Block())

            def run_stream(e, eng):
                waited = {}

                def wait(key, sem, val):
                    if waited.get(key, 0) >= val:
                        return
                    waited[key] = val
                    eng.wait_ge(sem, val)

                for op in self.ops[e]:
                    for d in op.deps:
                        if d.is_dma:
                            ep, slot = d.ring
                            wait(("r", d.eng, ep, slot), rings[d.eng][ep][slot], d.ringval[0])
                        else:
                            ep, v = d.seq
                            wait(("c", d.eng, ep), sems[d.eng][ep], v)
                    if op.fn is None:
                        continue
                    if op.is_dma:
                        ep, slot = op.ring
                        val, use = op.ringval
                        if use > 0:
                            pu = use - 1
                            wait(("r", e, pu // REP, slot), rings[e][pu // REP][slot], 16 * (pu % REP + 1))
                        op.fn(eng).then_inc(rings[e][ep][slot], 16)
                    else:
                        ins = op.fn(eng)
                        if op.has_dependents:
                            ins.then_inc(sems[e][op.seq[0]], 1)

            @block.tensor
            def _(eng):
                run_stream("pe", eng)

            @block.scalar
            def _(eng):
                run_stream("act", eng)

            @block.vector
            def _(eng):
                run_stream("dve", eng)

            @block.gpsimd
            def _(eng):
                run_stream("pool", eng)

            @block.sync
            def _(eng):
                run_stream("sync", eng)


class Arena:
    def __init__(self, t, nbytes):
        self.t = t
        self.nbytes = nbytes
        self.off = 0
        self.base = 0

    def reset(self):
        self.off = self.base

    def alloc(self, free_shape, dt, parts=128):
        esz = 4 if dt in (F32, I32) else 2
        n = int(np.prod(free_shape))
        nb = (n * esz + 63) // 64 * 64
        assert self.off + nb <= self.nbytes, ("SBUF arena overflow", self.off, nb, self.nbytes)
        v = self.t[0:parts, self.off // 2:(self.off + n * esz) // 2]
        self.off += nb
        if esz == 4:
            v = v.bitcast(dt)
        if len(free_shape) == 2:
            v = v.rearrange("p (a b) -> p a b", a=free_shape[0])
        elif len(free_shape) == 3:
            v = v.rearrange("p (a b c) -> p a b c", a=free_shape[0], b=free_shape[1])
        return v, Buf()


def build(SEQ, PAST, stop_after=None):
    nc = bass.Bass("TRN2", target_bir_lowering=False)
    P = Prog(nc)
    NTP = SEQ // 128
    NTOK = 2 * SEQ + 2 * NQ
    NT = NTOK // 128
    TKS = PAST + NQ
    NKBC = PAST // 128

    def din(name, shape, dt=F32):
        return nc.dram_tensor(name, list(shape), dt, kind="ExternalInput").ap()

    def dout(name, shape, dt=F32):
        return nc.dram_tensor(name, list(shape), dt, kind="ExternalOutput").ap()

    def dscr(name, shape, dt):
        return nc.dram_tensor(name, list(shape), dt, kind="Internal").ap(), Buf(name, multi=True)

    xp = din("xp", [2, SEQ, D]); xs = din("xs", [2, NQ, D])
    c_sbk = din("c_sbk", [2, PAST, 512]); c_sbv = din("c_sbv", [2, PAST, 512])
    c_fxk = din("c_fxk", [2, PAST, 512]); c_fxv = din("c_fxv", [2, PAST, 512])
    c_lf = din("c_lf", [2, PAST, 8])
    c_wk = din("c_wk", [2, 128, 128]); c_wv = din("c_wv", [2, 128, 128])
    g_ab = din("g_ab", [1, D]); w_in_ab = din("w_in_ab", [D, MIX_AB_IN]); b_fg = din("b_fg", [1, 8])
    w_out_ab = din("w_out_ab", [D, D]); g_ffn = din("g_ffn", [1, D])
    w_g = din("w_g", [D, D_FF]); w_u = din("w_u", [D, D_FF]); w_d = din("w_d", [D_FF, D])
    g_c = din("g_c", [1, D]); w_in_c = din("w_in_c", [D, MIX_C_IN]); sinks = din("sinks", [1, 16])
    w_out_c = din("w_out_c", [D, D]); g_moe = din("g_moe", [1, D]); w_r = din("w_r", [D, NEXP])
    w_ge = din("w_ge", [NEXP, D, D_EXP]); w_ue = din("w_ue", [NEXP, D, D_EXP]); w_de = din("w_de", [NEXP, D_EXP, D])
    g_fin = din("g_fin", [1, D])
    cst = din("cst", [128, 14, 128])
    alibi = din("alibi", [128, 16, 256])
    alibi2 = din("alibi2", [128, 2, 16, 256])
    iot = din("iot", [128, 64])

    y_p = dout("y_p", [2, SEQ, D]); y_s = dout("y_s", [2, NQ, D])
    o_p = {k: dout("p_" + k, [2, SEQ, 512]) for k in ("sbk", "sbv", "fxk", "fxv")}
    o_plf = dout("p_lf", [2, SEQ, 8])
    o_pwk = dout("p_wk", [2, 128, 128]); o_pwv = dout("p_wv", [2, 128, 128])
    o_s = {k: dout("s_" + k, [2, NQ, 512]) for k in ("sbk", "sbv", "fxk", "fxv")}
    o_slf = dout("s_lf", [2, NQ, 8])
    o_swk = dout("s_wk", [2, 128, 128]); o_swv = dout("s_wv", [2, 128, 128])
    OUTB = Buf("outs", multi=True)

    qt_p, B_qt_p = dscr("qt_p", [2, 2, 8, 64, SEQ], BF16)
    kt_p, B_kt_p = dscr("kt_p", [2, 2, 8, 64, SEQ], BF16)
    v_p, B_v_p = dscr("v_p", [2, 2, SEQ, 512], BF16)
    qt_s, B_qt_s = dscr("qt_s", [2, 2, 8, 64, NQ], BF16)
    kt_s, B_kt_s = dscr("kt_s", [2, 2, 8, 64, TKS], BF16)
    v_s, B_v_s = dscr("v_s", [2, 2, NQ, 512], BF16)
    c_p, B_c_p = dscr("c_p", [2, SEQ, 8], F32); ct_p, B_ct_p = dscr("ct_p", [2, 8, SEQ], F32)
    c_s, B_c_s = dscr("c_s", [2, TKS, 8], F32); ct_s, B_ct_s = dscr("ct_s", [2, 8, TKS], F32)
    ao_p, B_ao_p = dscr("ao_p", [2, 16, 64, SEQ], BF16)
    ao_s, B_ao_s = dscr("ao_s", [2, 16, 64, NQ], BF16)
    x1, B_x1 = dscr("x1", [NTOK, D], F32)
    h2t, B_h2t = dscr("h2t", [8, 128, NTOK], BF16)
    actt, B_actt = dscr("actt", [22, 128, NTOK], BF16)
    x2, B_x2 = dscr("x2", [NTOK, D], F32)
    qct, B_qct = dscr("qct", [16, 64, NTOK], BF16)
    kct, B_kct = dscr("kct", [2, 64, NTOK], BF16)
    vc, B_vc = dscr("vc", [NTOK, 128], BF16)
    x3, B_x3 = dscr("x3", [NTOK, D], F32)
    x4 = nc.dram_tensor("x4", [NTOK, D], F32, kind="Internal").ap()
    ao_c, B_aoc = dscr("aoc", [16, 64, NTOK], BF16)
    h4t, B_h4t = dscr("h4t", [8, 128, NTOK], BF16)
    h4k, B_h4k = dscr("h4k", [NTOK, D], BF16)
    NBLK = (2 * NTOK + NEXP * 511) // 512
    CAP = NBLK * 512
    assert NBLK <= 48
    xsl, B_xsl = dscr("xsl", [CAP, D], BF16)
    ysl, B_ysl = dscr("ysl", [CAP, D], F32)
    wge_s, B_wge = dscr("wge_s", [NEXP * 4 * 128, 8 * 896], BF16)
    wue_s, B_wue = dscr("wue_s", [NEXP * 4 * 128, 8 * 896], BF16)
    wde_s, B_wde = dscr("wde_s", [NEXP * 4 * 128, 7 * 1024], BF16)

    st = contextlib.ExitStack()
    ARB = 196 * 1024
    art = st.enter_context(nc.sbuf_tensor("arena", [128, ARB // 2], BF16))
    ar = Arena(art, ARB)
    pbanks = [st.enter_context(nc.psum_tensor(f"pb{i}", [128, 512], F32)) for i in range(8)]
    PB = [Buf(f"pb{i}", excl=True) for i in range(8)]

    def dma(q, out, in_, reads=(), writes=(), **kw):
        return P.add(q, lambda e, o=out, i=in_, k=kw: e.dma_start(out=o, in_=i, **k), reads, writes, is_dma=True)

    def mm(out, lhsT, rhs, start, stop, reads, writes):
        return P.add("pe", lambda e, o=out, l=lhsT, r=rhs, s=start, t=stop: e.matmul(o, lhsT=l, rhs=r, start=s, stop=t),
                     reads, writes)

    def tr(out, in_, ident, reads, writes):
        return P.add("pe", lambda e, o=out, i=in_, d=ident: e.transpose(out=o, in_=i, identity=d), reads, writes)

    def act(out, in_, func, reads, writes, bias=None, scale=None, accum=None):
        kw = {}
        if bias is not None:
            kw["bias"] = bias
        if scale is not None:
            kw["scale"] = scale
        if accum is not None:
            kw["accum_out"] = accum
        return P.add("act", lambda e, o=out, i=in_, f=func, k=kw: e.activation(out=o, in_=i, func=f, **k), reads, writes)

    def tt(eng, out, in0, in1, op, reads, writes):
        return P.add(eng, lambda e, o=out, a=in0, b=in1, p=op: e.tensor_tensor(out=o, in0=a, in1=b, op=p), reads, writes)

    def ts(eng, out, in0, s1, op0, reads, writes, s2=None, op1=None):
        if op1 is None:
            return P.add(eng, lambda e, o=out, a=in0, s=s1, p=op0: e.tensor_scalar(out=o, in0=a, scalar1=s, scalar2=None, op0=p),
                         reads, writes)
        return P.add(eng, lambda e, o=out, a=in0, s=s1, p=op0, s_2=s2, p1=op1:
                     e.tensor_scalar(out=o, in0=a, scalar1=s, scalar2=s_2, op0=p, op1=p1), reads, writes)

    def stt(out, in0, scalar, in1, op0, op1, reads, writes):
        return P.add("dve", lambda e, o=out, a=in0, s=scalar, b=in1, p0=op0, p1=op1:
                     e.scalar_tensor_tensor(out=o, in0=a, scalar=s, in1=b, op0=p0, op1=p1), reads, writes)

    def cp(eng, out, in_, reads, writes):
        if eng == "act":
            return act(out, in_, AF.Copy, reads, writes)
        return P.add(eng, lambda e, o=out, i=in_: e.tensor_copy(out=o, in_=i), reads, writes)

    def memset(eng, ap, val, writes):
        return P.add(eng, lambda e, a=ap, v=val: e.memset(a, v), (), writes)

    cf, B_cf = ar.alloc([14, 128], F32)
    cb, B_cb = ar.alloc([14, 128], BF16)
    dma("sync", cf, cst, writes=[B_cf])
    cp("dve", cb, cf, [B_cf], [B_cb])
    (C_ID, C_NUI, C_VSB, C_VFX, C_MSB, C_MFX, C_TRI, C_ONE, C_OLO, C_OHI, C_TRS, C_N64, C_TR2, C_SEL0) = range(14)
    iota_t, B_iota = ar.alloc([64], F32)
    dma("sync", iota_t, iot, writes=[B_iota])
    MK1, B_MK1 = ar.alloc([NT, NEXP], F32)
    MK2, B_MK2 = ar.alloc([NT, NEXP], F32)
    RANK, B_RK = ar.alloc([NT, NEXP], F32)
    G12, B_G12 = ar.alloc([NT, 2], F32)
    spm, spmB = ar.alloc([NEXP], F32)
    SL, B_SL = ar.alloc([NT, 2], I32)
    ar.base = ar.off
    CB = [B_cf, B_cb, B_iota]

    gtile = {}

    def load_gain(name, src):
        t, b = ar.alloc([D], F32)
        dma("sync", t, src[0:1, :].partition_broadcast(128).rearrange("p o d -> p (o d)"), writes=[b])
        gtile[name] = (t, b)

    def rmsnorm_to_bf16(x_ap, xb, g, hb, hbB, nrows, tmp):
        (sq, sqB), (ss, ssB), (rs, rsB) = tmp
        act(sq[0:nrows], x_ap, AF.Square, [xb], [sqB, ssB], accum=ss[0:nrows])
        act(rs[0:nrows], ss[0:nrows], AF.Ln, [ssB], [rsB], scale=1.0 / D, bias=EPS)
        act(rs[0:nrows], rs[0:nrows], AF.Exp, [rsB], [rsB], scale=-0.5)
        stt(hb, x_ap, rs[0:nrows, 0:1], g[0][0:nrows], ALU.mult, ALU.mult, [xb, rsB, g[1]], [hbB])

    def transpose_to(hb, hbB, dstT, dstB, col0, nrows, pbi):
        ptb = pbanks[pbi][:].bitcast(BF16)
        for kc in range(8):
            tr(ptb[:, kc * 128:kc * 128 + nrows], hb[0:nrows, kc * 128:(kc + 1) * 128], cb[0:nrows, C_ID, 0:nrows],
               [hbB, B_cb], [PB[pbi]])
        cp("act", dstT[:, :, col0:col0 + nrows], ptb.rearrange("p (k t) -> p k t", k=8)[:, :, 0:nrows], [PB[pbi]], [dstB])

    def x_rows(seq, t0, n):
        return xp[seq, t0:t0 + n, :]

    tiles = []
    for sq in range(2):
        for t0 in range(0, SEQ, 512):
            tiles.append(("p", sq, t0, 512, sq * SEQ + t0))
    tiles.append(("s", 0, 0, 128, 2 * SEQ))

    ar.reset()
    win, B_win = ar.alloc([8, 3136], BF16)
    for kc in range(8):
        dma("pool", win[:, kc, 0:MIX_AB_IN], w_in_ab[kc * 128:(kc + 1) * 128, :], writes=[B_win])
    load_gain("ab", g_ab)
    bfg, B_bfg = ar.alloc([8], F32)
    dma("sync", bfg, b_fg[0:1, :].partition_broadcast(128).rearrange("p o d -> p (o d)"), writes=[B_bfg])
    if stop_after == 'A0':
        P.barrier()
        return nc, P, st, locals()
    xt2 = [ar.alloc([4, D], F32) for _ in range(2)]
    hb_a = [ar.alloc([D], BF16) for _ in range(2)]
    hT_a = [ar.alloc([8, 512], BF16) for _ in range(2)]
    tmp_a = (ar.alloc([D], F32), ar.alloc([1], F32), ar.alloc([1], F32))
    qkst = [ar.alloc([512], BF16) for _ in range(4)]
    kvst = [ar.alloc([512], F32) for _ in range(4)]
    vbst = [ar.alloc([512], BF16) for _ in range(2)]
    sm = {k: ar.alloc([8], F32) for k in ("f", "e", "l", "lf", "c", "sp0", "sp1", "sps0", "sps1")}
    ctst = [ar.alloc([128], F32, parts=8) for _ in range(2)]
    ktst = [ar.alloc([512], BF16) for _ in range(2)]
    cach = [ar.alloc([512], BF16) for _ in range(2)]
    clft = [ar.alloc([8], F32) for _ in range(2)]

    def cumsum_tile(lf, lfB, nrows, sprev_list, dst_c, dst_cB, dst_ct, dst_ctB, i, sample_pair=False):
        pc = 6
        tri = cf[:, C_TR2 if sample_pair else C_TRI, :]
        ones = [cf[:, C_ONE, :]] if not sample_pair else [cf[:, C_OLO, :], cf[:, C_OHI, :]]
        mm(pbanks[pc][0:128, 0:8], tri, lf, True, False, [B_cf, lfB], [PB[pc]])
        for j, (sp, spB) in enumerate(sprev_list):
            mm(pbanks[pc][0:128, 0:8], ones[j], sp, False, j == len(sprev_list) - 1, [B_cf, spB], [PB[pc]])
        mm(pbanks[pc][0:8, 128:256], lf, tri, True, False, [B_cf, lfB], [PB[pc]])
        for j, (sp, spB) in enumerate(sprev_list):
            mm(pbanks[pc][0:8, 128:256], sp, ones[j], False, j == len(sprev_list) - 1, [B_cf, spB], [PB[pc]])
        ct, ctB = sm["c"]
        cp("dve", ct, pbanks[pc][0:128, 0:8], [PB[pc]], [ctB])
        ctt, cttB = ctst[i % 2]
        cp("dve", ctt, pbanks[pc][0:8, 128:256], [PB[pc]], [cttB])
        for (d_ap, rows) in dst_c:
            dma("sync", d_ap, ct[rows[0]:rows[1], :], reads=[ctB], writes=[dst_cB])
        for (d_ap, cols) in dst_ct:
            dma("sync", d_ap, ctt[:, cols[0]:cols[1]], reads=[cttB], writes=[dst_ctB])

    for sq in range(2):
        memset("dve", sm["sps%d" % sq][0], 0.0, [sm["sps%d" % sq][1]])

    def cache_step(sq, kb):
        sp, spB = sm["sps%d" % sq]
        if True:
            for ki, (csrc) in enumerate((c_sbk, c_fxk)):
                i = (kb * 2 + ki)
                ct_, cB_ = cach[i % 2]
                dma("pool", ct_, csrc[sq, kb * 128:(kb + 1) * 128, :], writes=[cB_])
                ptb = pbanks[7][:].bitcast(BF16)
                for j in range(4):
                    tr(ptb[:, j * 128:(j + 1) * 128], ct_[:, j * 128:(j + 1) * 128], cb[:, C_ID, :], [cB_, B_cb], [PB[7]])
                kt_, kB_ = ktst[i % 2]
                cp("act" if i % 2 else "dve", kt_, ptb[:, 0:512], [PB[7]], [kB_])
                for j in range(4):
                    dma("sync", kt_s[sq, ki, 2 * j:2 * j + 2, :, kb * 128:(kb + 1) * 128].rearrange("h d t -> (h d) t"),
                        kt_[:, j * 128:(j + 1) * 128], reads=[kB_], writes=[B_kt_s])
            lt, lB = clft[kb % 2]
            dma("sync", lt, c_lf[sq, kb * 128:(kb + 1) * 128, :], writes=[lB])
            cumsum_tile(lt, lB, 128, [(sp, spB)], [(c_s[sq, kb * 128:(kb + 1) * 128, :], (0, 128))], B_c_s,
                        [(ct_s[sq, :, kb * 128:(kb + 1) * 128], (0, 128))], B_ct_s, kb)
            tt("dve", sp, sp, lt, ALU.add, [spB, lB], [spB])

    cache_steps = [(sq_, kb_) for sq_ in range(2) for kb_ in range(NKBC)]
    n_ptiles = sum(1 for t_ in tiles if t_[0] == "p")
    cs_per = -(-len(cache_steps) // n_ptiles)
    cs_done = [0]

    def emit_cache_steps(n):
        for _ in range(n):
            if cs_done[0] < len(cache_steps):
                cache_step(*cache_steps[cs_done[0]])
                cs_done[0] += 1

    if stop_after == 'A1':
        P.barrier()
        return nc, P, st, locals()
    import os as _os
    DBG = _os.environ.get("DBG", "").split(",")
    def a_load(ti):
        kind, sq, t0, TW, g0 = tiles[ti]
        xt, xB = xt2[ti % 2]
        if kind == "p":
            dma("sync", xt[:, 0:TW // 128, :], xp[sq, t0:t0 + TW, :].rearrange("(s p) d -> p s d", p=128), writes=[xB])
        else:
            dma("sync", xt[0:64, 0, :], xs[0], writes=[xB])
            dma("sync", xt[64:128, 0, :], xs[1], writes=[xB])

    def a_norm(ti):
        kind, sq, t0, TW, g0 = tiles[ti]
        xt, xB = xt2[ti % 2]
        hT, hTB = hT_a[ti % 2]
        for s in range(TW // 128):
            hb, hbB = hb_a[s % 2]
            rmsnorm_to_bf16(xt[:, s, :], xB, gtile["ab"], hb, hbB, 128, tmp_a)
            transpose_to(hb, hbB, hT, hTB, s * 128, 128, 7)

    a_load(0)
    if len(tiles) > 1:
        a_load(1)
    a_norm(0)
    for ti, (kind, sq, t0, TW, g0) in enumerate(tiles):
        nsub = TW // 128
        xt, xB = xt2[ti % 2]
        hT, hTB = hT_a[ti % 2]
        if ti + 1 < len(tiles):
            a_norm(ti + 1)
        if ti + 2 < len(tiles):
            a_load(ti + 2)
        if kind == "p" and t0 == 0:
            memset("dve", sm["sp0"][0], 0.0, [sm["sp0"][1]])
        emit_cache_steps(cs_per if kind == "p" else len(cache_steps))
        fm = [("q", 0, 0), ("k", 0, 512), ("q", 1, 1536), ("k", 1, 2048)]
        ei = 0
        for (qk, sbfx, col0) in fm:
            if "fm" in DBG:
                break
            for j in range(4):
                pbi = ei % 4
                for kc in range(8):
                    mm(pbanks[pbi][:, 0:TW], win[:, kc, col0 + j * 128:col0 + (j + 1) * 128], hT[:, kc, 0:TW],
                       kc == 0, kc == 7, [B_win, hTB], [PB[pbi]])
                stt_, stB = qkst[ei % 4]
                if qk == "q":
                    act(stt_[:, 0:TW], pbanks[pbi][:, 0:TW], AF.Copy, [PB[pbi]], [stB], scale=0.125)
                else:
                    cp("dve", stt_[:, 0:TW], pbanks[pbi][:, 0:TW], [PB[pbi]], [stB])
                if kind == "p":
                    dstt, dB = (qt_p, B_qt_p) if qk == "q" else (kt_p, B_kt_p)
                    dma("sync", dstt[sq, sbfx, 2 * j:2 * j + 2, :, t0:t0 + TW].rearrange("h d t -> (h d) t"),
                        stt_[:, 0:TW], reads=[stB], writes=[dB])
                else:
                    for s2 in range(2):
                        if qk == "q":
                            dma("sync", qt_s[s2, sbfx, 2 * j:2 * j + 2, :, :].rearrange("h d t -> (h d) t"),
                                stt_[:, s2 * 64:(s2 + 1) * 64], reads=[stB], writes=[B_qt_s])
                        else:
                            dma("sync", kt_s[s2, sbfx, 2 * j:2 * j + 2, :, PAST:PAST + NQ].rearrange("h d t -> (h d) t"),
                                stt_[:, s2 * 64:(s2 + 1) * 64], reads=[stB], writes=[B_kt_s])
                ei += 1
        for s in range(nsub):
            for bi, (nm, col0, isv, sbfx) in enumerate((("sbk", 512, False, 0), ("sbv", 1024, True, 0),
                                                        ("fxk", 2048, False, 1), ("fxv", 2560, True, 1))):
                if "tm" in DBG:
                    break
                pbi = 4 + bi % 2
                for kc in range(8):
                    mm(pbanks[pbi][:, :], hT[:, kc, s * 128:(s + 1) * 128], win[:, kc, col0:col0 + 512],
                       kc == 0, kc == 7, [B_win, hTB], [PB[pbi]])
                kvt, kvB = kvst[bi]
                cp("act" if bi % 2 else "dve", kvt, pbanks[pbi][:, :], [PB[pbi]], [kvB])
                if isv:
                    vbt, vbB = vbst[bi // 2]
                    cp("dve" if bi % 2 else "act", vbt, pbanks[pbi][:, :], [PB[pbi]], [vbB])
                if kind == "p":
                    dma("sync", o_p[nm][sq, t0 + s * 128:t0 + (s + 1) * 128, :], kvt, reads=[kvB], writes=[OUTB])
                    if isv:
                        dma("sync", v_p[sq, sbfx, t0 + s * 128:t0 + (s + 1) * 128, :], vbt, reads=[vbB], writes=[B_v_p])
                else:
                    for s2 in range(2):
                        dma("sync", o_s[nm][s2], kvt[s2 * 64:(s2 + 1) * 64, :], reads=[kvB], writes=[OUTB])
                        if isv:
                            dma("sync", v_s[s2, sbfx], vbt[s2 * 64:(s2 + 1) * 64, :], reads=[vbB], writes=[B_v_s])
            if "fg" in DBG:
                continue
            for kc in range(8):
                mm(pbanks[6][:, 256:264], hT[:, kc, s * 128:(s + 1) * 128], win[:, kc, 3072:3080], kc == 0, kc == 7,
                   [B_win, hTB], [PB[6]])
            (f_, fB), (e_, eB), (l_, lB), (lf_, lfB) = sm["f"], sm["e"], sm["l"], sm["lf"]
            tt("dve", f_, pbanks[6][:, 256:264], bfg, ALU.add, [PB[6], B_bfg], [fB])
            act(e_, f_, AF.Exp, [fB], [eB], scale=-1.0)
            act(l_, e_, AF.Ln, [eB], [lB], bias=1.0)
            ts("dve", lf_, l_, -1.0, ALU.mult, [lB], [lfB])
            if kind == "p":
                r0 = t0 + s * 128
                dma("sync", o_plf[sq, r0:r0 + 128, :], lf_, reads=[lfB], writes=[OUTB])
                sp, spB = sm["sp0"]
                cumsum_tile(lf_, lfB, 128, [(sp, spB)], [(c_p[sq, r0:r0 + 128, :], (0, 128))], B_c_p,
                            [(ct_p[sq, :, r0:r0 + 128], (0, 128))], B_ct_p, s)
                tt("dve", sp, sp, lf_, ALU.add, [spB, lfB], [spB])
            else:
                for s2 in range(2):
                    dma("sync", o_slf[s2], lf_[s2 * 64:(s2 + 1) * 64, :], reads=[lfB], writes=[OUTB])
                cumsum_tile(lf_, lfB, 128, [sm["sps0"], sm["sps1"]],
                            [(c_s[0, PAST:PAST + NQ, :], (0, 64)), (c_s[1, PAST:PAST + NQ, :], (64, 128))], B_c_s,
                            [(ct_s[0, :, PAST:PAST + NQ], (0, 64)), (ct_s[1, :, PAST:PAST + NQ], (64, 128))], B_ct_s, s,
                            sample_pair=True)
    P.barrier()
    if stop_after == 'A':
        return nc, P, st, locals()

    ar.reset()
    TKM = max(SEQ, TKS)
    NKM = TKM // 128 + 1
    ktA = [ar.alloc([TKM], BF16) for _ in range(2)]
    ktF = [ar.alloc([TKM], BF16) for _ in range(2)]
    qtA = [ar.alloc([SEQ], BF16) for _ in range(2)]
    qtF = [ar.alloc([SEQ], BF16) for _ in range(2)]
    qrowB = [Buf() for _ in range(2)]
    vA = [ar.alloc([NKM, 64], BF16) for _ in range(2)]
    vF = [ar.alloc([NKM, 128], BF16) for _ in range(2)]
    crow = [ar.alloc([SEQ], F32) for _ in range(2)]
    cref = [ar.alloc([8], F32) for _ in range(2)]
    ctok, B_ctok = ar.alloc([NKM, 8], F32)
    nbias = [ar.alloc([NKM], F32) for _ in range(2)]
    e_t = [ar.alloc([512], F32) for _ in range(2)]
    l_t = [ar.alloc([512], F32) for _ in range(2)]
    lp_t = [ar.alloc([512], BF16) for _ in range(2)]
    w_t = [ar.alloc([512], BF16) for _ in range(2)]
    aost = [ar.alloc([512], BF16) for _ in range(2)]
    rD_t, B_rD = ar.alloc([512], F32)
    zt, B_zt = ar.alloc([512], BF16)
    r32, B_r32 = ar.alloc([512], F32)
    memset("dve", zt, 0.0, [B_zt])
    for i in range(2):
        memset("dve", ktA[i][0][64:65, :], 1.0, [ktA[i][1]])
        memset("dve", ktF[i][0][64:65, :], 1.0, [ktF[i][1]])
        memset("pool", vF[i][0][:, :, 64:128], 1.0, [vF[i][1]])
    wf_t = [ar.alloc([512], BF16) for _ in range(2)]
    pAs = [0, 6]; pAf = 1; pBk = [2, 3]; pO = 4; pR = 5; pOf = 7

    def zero_bank(pbi, rows, ncols):
        mm(pbanks[pbi][0:rows, 0:ncols], zt[:, 0:rows], zt[:, 0:ncols], True, False, [B_zt], [PB[pbi]])

    def seq_params(sidx):
        is_p = sidx < 2
        sq = sidx % 2
        if is_p:
            groups = []
            for qb in range(SEQ // 512):
                tl = [(4 * qb + j, 128, 128 * j, True) for j in (3, 2, 1, 0)] + [(kb, 128, 0, False) for kb in range(4 * qb - 1, -1, -1)]
                groups.append((512 * qb, 512, 512 * qb, tl))
            return dict(is_p=True, sq=sq, QTs=qt_p[sq], KTs=kt_p[sq], Cs=c_p[sq], CTs=ct_p[sq], AOs=ao_p[sq], TQ=SEQ, TK=SEQ,
                        BQ=B_qt_p, BK=B_kt_p, BC=B_c_p, BCT=B_ct_p, BAO=B_ao_p, groups=groups, nkb_all=SEQ // 128)
        groups = [(0, NQ, PAST, [(NKBC, 64, 0, True)] + [(kb, 128, 0, False) for kb in range(NKBC - 1, -1, -1)])]
        return dict(is_p=False, sq=sq, QTs=qt_s[sq], KTs=kt_s[sq], Cs=c_s[sq], CTs=ct_s[sq], AOs=ao_s[sq], TQ=NQ, TK=TKS,
                    BQ=B_qt_s, BK=B_kt_s, BC=B_c_s, BCT=B_ct_s, BAO=B_ao_s, groups=groups, nkb_all=NKBC)

    def load_head(sp, h, a2):
        (kA, kAB), (kF, kFB), (qA, qAB), (qF, qFB) = ktA[a2], ktF[a2], qtA[a2], qtF[a2]
        (vAt, vAB), (vFt, vFB), (cr, crB), (crf, crfB) = vA[a2], vF[a2], crow[a2], cref[a2]
        sq, TK, TQ, KTs, QTs, CTs = sp["sq"], sp["TK"], sp["TQ"], sp["KTs"], sp["QTs"], sp["CTs"]
        BK, BQ, BCT, nkb_all = sp["BK"], sp["BQ"], sp["BCT"], sp["nkb_all"]
        dma("sync", kA[0:64, 0:TK], KTs[0, h], reads=[BK], writes=[kAB])
        dma("sync", kF[0:64, 0:TK], KTs[1, h], reads=[BK], writes=[kFB])
        dma("sync", qA[0:64, 0:TQ], QTs[0, h], reads=[BQ], writes=[qAB])
        dma("sync", qF[0:64, 0:TQ], QTs[1, h], reads=[BQ], writes=[qFB])
        if sp["is_p"]:
            for (vt, vB, ki) in ((vAt, vAB, 0), (vFt, vFB, 1)):
                dma("sync", vt[:, 0:nkb_all, 0:64], v_p[sq, ki, :, h * 64:(h + 1) * 64].rearrange("(k p) d -> p k d", p=128),
                    reads=[B_v_p], writes=[vB])
            dma("sync", cr[64:65, 0:SEQ], CTs[h:h + 1, :], reads=[BCT], writes=[crB])
            dma("sync", crf[:, 0:SEQ // 512],
                CTs[h:h + 1, 0:SEQ:512].partition_broadcast(128).rearrange("p o n -> p (o n)"), reads=[BCT], writes=[crfB],
                allow_slow_non_contiguous=True)
        else:
            for (vt, vB, ki, csrc) in ((vAt, vAB, 0, c_sbv), (vFt, vFB, 1, c_fxv)):
                dma("pool", vt[:, 0:NKBC, 0:64], csrc[sq, :, h * 64:(h + 1) * 64].rearrange("(k p) d -> p k d", p=128), writes=[vB])
                dma("sync", vt[0:64, NKBC, 0:64], v_s[sq, ki, :, h * 64:(h + 1) * 64], reads=[B_v_s], writes=[vB])
            dma("sync", cr[64:65, 0:NQ], CTs[h:h + 1, PAST:PAST + NQ], reads=[BCT], writes=[crB])
            dma("sync", crf[:, 0:1], CTs[h:h + 1, PAST:PAST + 1].partition_broadcast(128).rearrange("p o n -> p (o n)"),
                reads=[BCT], writes=[crfB])

    stg = [ar.alloc([7168], BF16) for _ in range(2)]
    chunks = [(e, q, mat) for e in range(NEXP) for q in range(4) for mat in range(3)]

    def precast_chunk(ci):
        e, q, mat = chunks[ci]
        t, b = stg[ci % 2]
        if mat < 2:
            src = (w_ge, w_ue)[mat][e].rearrange("(k p) m -> p k m", p=128)[:, :, q * 896:(q + 1) * 896]
            dma("pool", t[:, 0:7168].rearrange("p (k m) -> p k m", k=8), src, writes=[b])
            dst, dB = (wge_s, wue_s)[mat], (B_wge, B_wue)[mat]
        else:
            src = w_de[e, q * 896:(q + 1) * 896, :].rearrange("(m p) n -> p m n", p=128)
            dma("pool", t[:, 0:7168].rearrange("p (m n) -> p m n", m=7), src, writes=[b])
            dst, dB = wde_s, B_wde
        r0 = (e * 4 + q) * 128
        dma("sync", dst[r0:r0 + 128, :], t[:, 0:7168], reads=[b], writes=[dB])

    heads = [(sidx, h) for sidx in range(4) for h in range(8)]
    sps = [seq_params(sidx) for sidx in range(4)]
    aoc = 0
    load_head(sps[0], 0, 0)
    for hi, (sidx, h) in enumerate(heads):
        sp = sps[sidx]
        is_p, sq, Cs, AOs, BC, BAO, groups, nkb_all = sp["is_p"], sp["sq"], sp["Cs"], sp["AOs"], sp["BC"], sp["BAO"], sp["groups"], sp["nkb_all"]
        if h == 0:
            dma("sync", ctok[:, 0:nkb_all, :], Cs[0:nkb_all * 128, :].rearrange("(k p) h -> p k h", p=128), reads=[BC], writes=[B_ctok])
            if not is_p:
                dma("sync", ctok[0:64, NKBC, :], Cs[PAST:PAST + NQ, :], reads=[BC], writes=[B_ctok])
        if hi + 1 < len(heads):
            load_head(sps[heads[hi + 1][0]], heads[hi + 1][1], (hi + 1) % 2)
        for ci in range(3 * hi, 3 * hi + 3):
            precast_chunk(ci)
        a2 = hi % 2
        (kA, kAB), (kF, kFB), (qA, qAB), (qF, qFB) = ktA[a2], ktF[a2], qtA[a2], qtF[a2]
        (vAt, vAB), (vFt, vFB), (cr, crB), (crf, crfB) = vA[a2], vF[a2], crow[a2], cref[a2]
        qrB = qrowB[a2]
        for gi, (q0, ncols, cq0, tl) in enumerate(groups):
            n = len(tl)

            def tpar(i):
                kb, nk, c0, diag = tl[i]
                return kb, nk, c0, diag, slice(kb * 128, kb * 128 + nk), slice(q0 + c0, q0 + ncols), slice(c0, ncols)

            zero_bank(pO, 64, ncols)
            memset("dve", qA[64:65, q0:q0 + ncols], 0.0, [qrB])
            memset("pool", r32[64:65, 0:ncols], 0.0, [B_r32])
            zero_bank(pOf, 128, ncols)
            cq = cq0 if is_p else 0
            ts("dve", qF[64:65, q0:q0 + ncols], cr[64:65, cq:cq + ncols], cr[64:65, cq:cq + 1], ALU.subtract, [crB], [qFB])
            (nb, nbB) = nbias[gi % 2]
            ts("dve", nb[:, 0:nkb_all + 1], ctok[:, 0:nkb_all + 1, h], crf[:, gi:gi + 1], ALU.subtract, [B_ctok, crfB], [nbB],
               s2=-1.0, op1=ALU.mult)

            def sb_A(i):
                kb, nk, c0, diag, ks, qs, cs = tpar(i)
                a = i % 2
                (et, eB), (lt, lB), (lpt, lpB) = e_t[a], l_t[a], lp_t[a]
                mm(pbanks[pAs[i % 2]][0:nk, cs], kA[0:64, ks], qA[0:64, qs], True, True, [kAB, qAB], [PB[pAs[i % 2]]])
                act(et[0:nk, cs], pbanks[pAs[i % 2]][0:nk, cs], AF.Exp, [PB[pAs[i % 2]]], [eB], scale=-1.0)
                act(lt[0:nk, cs], et[0:nk, cs], AF.Ln, [eB], [lB], bias=1.0)
                tt("dve", lpt[0:nk, cs], pbanks[pAs[i % 2]][0:nk, cs], lt[0:nk, cs], ALU.add, [PB[pAs[i % 2]], lB], [lpB])
                if diag:
                    tt("dve", lpt[0:nk, c0:c0 + nk], lpt[0:nk, c0:c0 + nk], cb[0:nk, C_VSB, 0:nk], ALU.mult, [lpB, B_cb], [lpB])

            def sb_R(i):
                kb, nk, c0, diag, ks, qs, cs = tpar(i)
                (lpt, lpB) = lp_t[i % 2]
                mm(pbanks[pR][0:65, cs], cb[0:nk, C_N64, 0:65], lpt[0:nk, cs], True, True, [B_cb, lpB], [PB[pR]])
                tt("dve", r32[64:65, cs], pbanks[pR][64:65, cs], r32[64:65, cs], ALU.add, [PB[pR], B_r32], [B_r32])

            def sb_B(i):
                kb, nk, c0, diag, ks, qs, cs = tpar(i)
                a = i % 2
                (lpt, lpB), (wt, wB) = lp_t[a], w_t[a]
                mm(pbanks[pBk[a]][0:nk, cs], kA[0:65, ks], qA[0:65, qs], True, False, [kAB, qAB, qrB], [PB[pBk[a]]])
                if diag:
                    mm(pbanks[pBk[a]][0:nk, c0:c0 + nk], cb[0:nk, C_ID, 0:nk], cb[0:nk, C_MSB, 0:nk], False, False, [B_cb], [PB[pBk[a]]])
                mm(pbanks[pBk[a]][0:nk, cs], cb[0:nk, C_NUI, 0:nk], lpt[0:nk, cs], False, True, [B_cb, lpB], [PB[pBk[a]]])
                act(wt[0:nk, cs], pbanks[pBk[a]][0:nk, cs], AF.Exp, [PB[pBk[a]]], [wB])
                if i + 1 < n:
                    cp("pool", qA[64:65, q0 + c0:q0 + ncols], r32[64:65, cs], [B_r32], [qrB])

            def sb_O(i):
                kb, nk, c0, diag, ks, qs, cs = tpar(i)
                (wt, wB) = w_t[i % 2]
                mm(pbanks[pO][0:64, cs], vAt[0:nk, kb, :], wt[0:nk, cs], False, i + 1 == n, [vAB, wB], [PB[pO]])

            def fx_A(i):
                kb, nk, c0, diag, ks, qs, cs = tpar(i)
                (wt, wB) = wf_t[i % 2]
                mm(pbanks[pAf][0:nk, cs], kF[0:65, ks], qF[0:65, qs], True, not diag, [kFB, qFB], [PB[pAf]])
                if diag:
                    mm(pbanks[pAf][0:nk, c0:c0 + nk], cb[0:nk, C_ID, 0:nk], cb[0:nk, C_MFX, 0:nk], False, True, [B_cb], [PB[pAf]])
                act(wt[0:nk, cs], pbanks[pAf][0:nk, cs], AF.Exp, [PB[pAf], nbB], [wB], bias=nb[0:nk, kb:kb + 1])

            def fx_OD(i):
                kb, nk, c0, diag, ks, qs, cs = tpar(i)
                (wt, wB) = wf_t[i % 2]
                mm(pbanks[pOf][0:128, cs], vFt[0:nk, kb, :], wt[0:nk, cs], False, i + 1 == n, [vFB, wB], [PB[pOf]])

            for k in range(n + 2):
                if k < n:
                    sb_A(k)
                    fx_A(k)
                if 0 <= k - 2 < n:
                    sb_O(k - 2)
                if 0 <= k - 1 < n:
                    fx_OD(k - 1)
                    if k < n:
                        sb_R(k - 1)
                    sb_B(k - 1)
            (ao, aoB) = aost[aoc % 2]; aoc += 1
            cp("act", ao[0:64, 0:ncols], pbanks[pO][0:64, 0:ncols], [PB[pO]], [aoB])
            dma("sync", AOs[h, :, q0:q0 + ncols], ao[0:64, 0:ncols], reads=[aoB], writes=[BAO])
            P.add("dve", lambda e, o=rD_t[64:128, 0:ncols], i_=pbanks[pOf][64:128, 0:ncols]: e.reciprocal(out=o, in_=i_), [PB[pOf]], [B_rD])
            (ao, aoB) = aost[aoc % 2]; aoc += 1
            tt("dve", ao[0:64, 0:ncols], pbanks[pOf][0:64, 0:ncols], rD_t[64:128, 0:ncols], ALU.mult, [PB[pOf], B_rD], [aoB])
            dma("sync", AOs[8 + h, :, q0:q0 + ncols], ao[0:64, 0:ncols], reads=[aoB], writes=[BAO])
    P.barrier()
    if stop_after == 'B':
        return nc, P, st, locals()

    AX = mybir.AxisListType.X

    def reduce_max(out, in_, reads, writes):
        return P.add("dve", lambda e, o=out, i=in_: e.tensor_reduce(out=o, in_=i, axis=AX, op=ALU.max), reads, writes)

    def recip(out, in_, reads, writes):
        return P.add("dve", lambda e, o=out, i=in_: e.reciprocal(out=o, in_=i), reads, writes)

    def ao_view(src3):
        return src3.rearrange("(k h) d t -> (h d) k t", h=2)

    def phase_outproj(name, ao_loader, x_loader, w_out_d, gain_d, x_dst, xdB, hT_dst, hTdB, router=False):
        ar.reset()
        wo, woB = ar.alloc([8, D], BF16)
        for kc in range(8):
            dma("pool", wo[:, kc, :], w_out_d[kc * 128:(kc + 1) * 128, :], writes=[woB])
        load_gain(name, gain_d)
        aoT = [ar.alloc([8, 512], BF16) for _ in range(2)]
        xts = [ar.alloc([4, D], F32) for _ in range(2)]
        hbs = [ar.alloc([D], BF16) for _ in range(2)]
        hTs = [ar.alloc([8, 512], BF16) for _ in range(2)]
        tmp = (ar.alloc([D], F32), ar.alloc([1], F32), ar.alloc([1], F32))
        if router:
            wr_t, wrB = ar.alloc([8, NEXP], F32)
            dma("sync", wr_t, w_r.rearrange("(k p) e -> p k e", p=128), writes=[wrB])
            hfs = [ar.alloc([D], F32) for _ in range(2)]
            hfT, hfTB = ar.alloc([8, 128], F32)
            r = {k: ar.alloc([8], F32) for k in ("lg", "mk1", "l2")}
            memset("dve", spm, 0.0, [spmB])
            r1 = {k: ar.alloc([1], F32) for k in ("m1", "m2", "dd", "ee", "dn", "g1", "g2")}

        def loads(ti):
            ao_loader(tiles[ti], aoT[ti % 2])
            x_loader(tiles[ti], xts[ti % 2])

        loads(0)
        for ti, (kind, sq, t0, TW, g0) in enumerate(tiles):
            if ti + 1 < len(tiles):
                loads(ti + 1)
            at, atB = aoT[ti % 2]
            xt, xB = xts[ti % 2]
            hT, hTB = hTs[ti % 2]
            nsub = TW // 128
            def stage1(s, ti=ti, g0=g0, at=at, atB=atB, xt=xt, xB=xB, hT=hT, hTB=hTB):
                for half in range(2):
                    pbi = (2 * s + half) % 4
                    for kc in range(8):
                        mm(pbanks[pbi][:, :], at[:, kc, s * 128:(s + 1) * 128], wo[:, kc, half * 512:(half + 1) * 512],
                           kc == 0, kc == 7, [atB, woB], [PB[pbi]])
                    tt("dve", xt[:, s, half * 512:(half + 1) * 512], xt[:, s, half * 512:(half + 1) * 512], pbanks[pbi][:, :],
                       ALU.add, [xB, PB[pbi]], [xB])
                hb, hbB = hbs[s % 2]
                rmsnorm_to_bf16(xt[:, s, :], xB, gtile[name], hb, hbB, 128, tmp)
                if not router:
                    transpose_to(hb, hbB, hT, hTB, s * 128, 128, 7)
                if router:
                    (rs, rsB) = tmp[2]
                    g = gtile[name]
                    hf, hfB = hfs[s % 2]
                    stt(hf, xt[:, s, :], rs[:, 0:1], g[0], ALU.mult, ALU.mult, [xB, rsB, g[1]], [hfB])
                    dma("sync", h4k[g0 + s * 128:g0 + (s + 1) * 128, :], hb, reads=[hbB], writes=[B_h4k])
            def stage2(s, g0=g0):
                hf, hfB = hfs[s % 2]
                for kc in range(8):
                    pbi = 4 + kc // 4
                    tr(pbanks[pbi][:, (kc % 4) * 128:(kc % 4 + 1) * 128], hf[:, kc * 128:(kc + 1) * 128], cf[:, C_ID, :],
                       [hfB, B_cf], [PB[pbi]])
                cp("act", hfT[:, 0:4, :], pbanks[4][:, :].rearrange("p (k t) -> p k t", k=4), [PB[4]], [hfTB])
                cp("act", hfT[:, 4:8, :], pbanks[5][:, :].rearrange("p (k t) -> p k t", k=4), [PB[5]], [hfTB])
                for kc in range(8):
                    mm(pbanks[6][:, 0:8], hfT[:, kc, :], wr_t[:, kc, :], kc == 0, kc == 7, [hfTB, wrB], [PB[6]])
                st_ = g0 // 128 + s
                (lg, lgB), (l2, l2B), (msum, msumB) = r["lg"], r["l2"], r["mk1"]
                (m1, m1B), (m2, m2B), (dd, ddB), (ee, eeB) = r1["m1"], r1["m2"], r1["dd"], r1["ee"]
                (dn, dnB) = r1["dn"]
                mk1, mk2 = MK1[:, st_, :], MK2[:, st_, :]
                cp("dve", lg, pbanks[6][:, 0:8], [PB[6]], [lgB])
                reduce_max(m1, lg, [lgB], [m1B])
                ts("dve", mk1, lg, m1[:, 0:1], ALU.is_equal, [lgB, m1B], [B_MK1])
                stt(l2, mk1, -1e30, lg, ALU.mult, ALU.add, [B_MK1, lgB], [l2B])
                reduce_max(m2, l2, [l2B], [m2B])
                ts("dve", mk2, l2, m2[:, 0:1], ALU.is_equal, [l2B, m2B], [B_MK2])
                tt("dve", dd, m2, m1, ALU.subtract, [m1B, m2B], [ddB])
                act(ee, dd, AF.Exp, [ddB], [eeB])
                ts("dve", dn, ee, 1.0, ALU.add, [eeB], [dnB])
                recip(G12[:, st_, 0:1], dn, [dnB], [B_G12])
                tt("dve", G12[:, st_, 1:2], ee, G12[:, st_, 0:1], ALU.mult, [eeB, B_G12], [B_G12])
                tt("dve", msum, mk1, mk2, ALU.add, [B_MK1, B_MK2], [msumB])
                mm(pbanks[6][:, 8:16], cf[:, C_TRS, :], msum, True, False, [B_cf, msumB], [PB[6]])
                mm(pbanks[6][:, 8:16], cf[:, C_ONE, :], spm, False, True, [B_cf, spmB], [PB[6]])
                cp("dve", RANK[:, st_, :], pbanks[6][:, 8:16], [PB[6]], [B_RK])
                tt("dve", spm, spm, msum, ALU.add, [spmB, msumB], [spmB])
            for s in range(nsub + 1):
                if s < nsub:
                    stage1(s)
                if router and s >= 1:
                    stage2(s - 1)
            dma("sync", x_dst[g0:g0 + TW, :].rearrange("(s p) d -> p s d", p=128), xt[:, 0:nsub, :], reads=[xB], writes=[xdB])
            if not router:
                dma("sync", hT_dst[:, :, g0:g0 + TW].rearrange("k p t -> p k t"), hT[:, :, 0:TW], reads=[hTB], writes=[hTdB])
        P.barrier()

    def ao_loader0(tile, buf):
        kind, sq, t0, TW, g0 = tile
        t, b = buf
        if kind == "p":
            dma("sync", t[:, :, 0:TW], ao_view(ao_p[sq])[:, :, t0:t0 + TW], reads=[B_ao_p], writes=[b])
        else:
            for s2 in range(2):
                dma("sync", t[:, :, s2 * 64:(s2 + 1) * 64], ao_view(ao_s[s2]), reads=[B_ao_s], writes=[b])

    def x_loader0(tile, buf):
        kind, sq, t0, TW, g0 = tile
        t, b = buf
        if kind == "p":
            dma("sync", t[:, 0:TW // 128, :], xp[sq, t0:t0 + TW, :].rearrange("(s p) d -> p s d", p=128), writes=[b])
        else:
            dma("sync", t[0:64, 0, :], xs[0], writes=[b])
            dma("sync", t[64:128, 0, :], xs[1], writes=[b])

    phase_outproj("ffn", ao_loader0, x_loader0, w_out_ab, g_ffn, x1, B_x1, h2t, B_h2t)
    if stop_after == 'C1':
        return nc, P, st, locals()

    def gate_up(hT, hTB, TW, wg_t, wgB, wu_t, wuB, nm, aT, aTB, sgs):
        for m in range(nm):
            pg, pu = (0, 1) if m % 2 == 0 else (2, 3)
            for kc in range(8):
                mm(pbanks[pg][:, 0:TW], wg_t[:, kc, m * 128:(m + 1) * 128], hT[:, kc, 0:TW], kc == 0, kc == 7, [wgB, hTB], [PB[pg]])
            for kc in range(8):
                mm(pbanks[pu][:, 0:TW], wu_t[:, kc, m * 128:(m + 1) * 128], hT[:, kc, 0:TW], kc == 0, kc == 7, [wuB, hTB], [PB[pu]])
            sg, sgB = sgs[m % 2]
            act(sg[:, 0:TW], pbanks[pg][:, 0:TW], AF.Silu, [PB[pg]], [sgB])
            tt("dve", aT[:, m, 0:TW], sg[:, 0:TW], pbanks[pu][:, 0:TW], ALU.mult, [sgB, PB[pu]], [aTB])

    ar.reset()
    wg_t, wgB = ar.alloc([8, D_FF], BF16)
    wu_t, wuB = ar.alloc([8, D_FF], BF16)
    for kc in range(8):
        dma("pool", wg_t[:, kc, :], w_g[kc * 128:(kc + 1) * 128, :], writes=[wgB])
        dma("pool", wu_t[:, kc, :], w_u[kc * 128:(kc + 1) * 128, :], writes=[wuB])
    hTs = [ar.alloc([8, 512], BF16) for _ in range(2)]
    aTs = [ar.alloc([22, 512], BF16) for _ in range(2)]
    sgs = [ar.alloc([512], F32) for _ in range(2)]

    def ld_c2(ti):
        kind, sq, t0, TW, g0 = tiles[ti]
        dma("sync", hTs[ti % 2][0][:, :, 0:TW], h2t[:, :, g0:g0 + TW].rearrange("k p t -> p k t"), reads=[B_h2t], writes=[hTs[ti % 2][1]])

    ld_c2(0)
    for ti, (kind, sq, t0, TW, g0) in enumerate(tiles):
        if ti + 1 < len(tiles):
            ld_c2(ti + 1)
        hT, hTB = hTs[ti % 2]
        aT, aTB = aTs[ti % 2]
        gate_up(hT, hTB, TW, wg_t, wgB, wu_t, wuB, 22, aT, aTB, sgs)
        dma("sync", actt[:, :, g0:g0 + TW].rearrange("m p t -> p m t"), aT[:, :, 0:TW], reads=[aTB], writes=[B_actt])
    P.barrier()
    if stop_after == 'C2':
        return nc, P, st, locals()

    ar.reset()
    wd_t, wdB = ar.alloc([22, D], BF16)
    for m in range(22):
        dma("pool", wd_t[:, m, :], w_d[m * 128:(m + 1) * 128, :], writes=[wdB])
    winc, wincB = ar.alloc([8, MIX_C_IN], BF16)
    for kc in range(8):
        dma("pool", winc[:, kc, :], w_in_c[kc * 128:(kc + 1) * 128, :], writes=[wincB])
    load_gain("c", g_c)
    aTs = [ar.alloc([22, 512], BF16) for _ in range(2)]
    xts = [ar.alloc([4, D], F32) for _ in range(2)]
    hbs = [ar.alloc([D], BF16) for _ in range(2)]
    hTs = [ar.alloc([8, 512], BF16) for _ in range(2)]
    tmp = (ar.alloc([D], F32), ar.alloc([1], F32), ar.alloc([1], F32))
    qsts = [ar.alloc([512], BF16) for _ in range(2)]
    kvts = [ar.alloc([256], F32) for _ in range(2)]
    vbts = [ar.alloc([128], BF16) for _ in range(2)]
    cws = [ar.alloc([128], F32) for _ in range(2)]

    def ld_c3(ti):
        kind, sq, t0, TW, g0 = tiles[ti]
        dma("sync", aTs[ti % 2][0][:, :, 0:TW], actt[:, :, g0:g0 + TW].rearrange("m p t -> p m t"), reads=[B_actt], writes=[aTs[ti % 2][1]])
        dma("sync", xts[ti % 2][0][:, 0:TW // 128, :], x1[g0:g0 + TW, :].rearrange("(s p) d -> p s d", p=128), reads=[B_x1],
            writes=[xts[ti % 2][1]])

    ld_c3(0)
    qi = 0
    for ti, (kind, sq, t0, TW, g0) in enumerate(tiles):
        if ti + 1 < len(tiles):
            ld_c3(ti + 1)
        aT, aTB = aTs[ti % 2]
        xt, xB = xts[ti % 2]
        hT, hTB = hTs[ti % 2]
        nsub = TW // 128
        for s in range(nsub):
            for half in range(2):
                pbi = (2 * s + half) % 4
                for m in range(22):
                    mm(pbanks[pbi][:, :], aT[:, m, s * 128:(s + 1) * 128], wd_t[:, m, half * 512:(half + 1) * 512],
                       m == 0, m == 21, [aTB, wdB], [PB[pbi]])
                tt("dve", xt[:, s, half * 512:(half + 1) * 512], xt[:, s, half * 512:(half + 1) * 512], pbanks[pbi][:, :],
                   ALU.add, [xB, PB[pbi]], [xB])
            hb, hbB = hbs[s % 2]
            rmsnorm_to_bf16(xt[:, s, :], xB, gtile["c"], hb, hbB, 128, tmp)
            transpose_to(hb, hbB, hT, hTB, s * 128, 128, 7)
        dma("sync", x2[g0:g0 + TW, :].rearrange("(s p) d -> p s d", p=128), xt[:, 0:nsub, :], reads=[xB], writes=[B_x2])
        for j in range(9):
            pbi = 4 + j % 2
            c0 = j * 128
            for kc in range(8):
                mm(pbanks[pbi][:, 0:TW], winc[:, kc, c0:c0 + 128], hT[:, kc, 0:TW], kc == 0, kc == 7, [wincB, hTB], [PB[pbi]])
            qs, qsB = qsts[qi % 2]; qi += 1
            if j < 8:
                act(qs[:, 0:TW], pbanks[pbi][:, 0:TW], AF.Copy, [PB[pbi]], [qsB], scale=0.125)
                dma("sync", qct[2 * j:2 * j + 2, :, g0:g0 + TW].rearrange("h d t -> (h d) t"), qs[:, 0:TW], reads=[qsB], writes=[B_qct])
            else:
                cp("dve", qs[:, 0:TW], pbanks[pbi][:, 0:TW], [PB[pbi]], [qsB])
                dma("sync", kct[0:2, :, g0:g0 + TW].rearrange("h d t -> (h d) t"), qs[:, 0:TW], reads=[qsB], writes=[B_kct])
        for s in range(nsub):
            pbi = 6
            for kc in range(8):
                mm(pbanks[pbi][:, 0:256], hT[:, kc, s * 128:(s + 1) * 128], winc[:, kc, 1024:1280], kc == 0, kc == 7,
                   [wincB, hTB], [PB[pbi]])
            kvt, kvB = kvts[s % 2]
            vbt, vbB = vbts[s % 2]
            cp("act", kvt, pbanks[pbi][:, 0:256], [PB[pbi]], [kvB])
            cp("dve", vbt, pbanks[pbi][:, 128:256], [PB[pbi]], [vbB])
            dma("sync", vc[g0 + s * 128:g0 + (s + 1) * 128, :], vbt, reads=[vbB], writes=[B_vc])
            if kind == "p":
                if t0 + s * 128 == SEQ - 128:
                    dma("sync", o_pwk[sq], kvt[:, 0:128], reads=[kvB], writes=[OUTB])
                    dma("sync", o_pwv[sq], kvt[:, 128:256], reads=[kvB], writes=[OUTB])
            else:
                for s2 in range(2):
                    dma("sync", o_swk[s2, 64:128, :], kvt[s2 * 64:(s2 + 1) * 64, 0:128], reads=[kvB], writes=[OUTB])
                    dma("sync", o_swv[s2, 64:128, :], kvt[s2 * 64:(s2 + 1) * 64, 128:256], reads=[kvB], writes=[OUTB])
                    for ci, (csrc, odst) in enumerate(((c_wk, o_swk), (c_wv, o_swv))):
                        cw, cwB = cws[ci]
                        dma("sync", cw[0:64, :], csrc[s2, 64:128, :], writes=[cwB])
                        dma("sync", odst[s2, 0:64, :], cw[0:64, :], reads=[cwB], writes=[OUTB])
    P.barrier()
    if stop_after == 'C3':
        return nc, P, st, locals()

    ar.reset()
    alib, alibB = ar.alloc([16, 256], F32)
    dma("sync", alib, alibi, writes=[alibB])
    esk, eskB = ar.alloc([16], F32)
    dma("sync", esk, sinks[0:1, :].partition_broadcast(128).rearrange("p o d -> p (o d)"), writes=[eskB])
    act(esk, esk, AF.Exp, [eskB], [eskB])
    kTq = [ar.alloc([2, SEQ], BF16, parts=64) for _ in range(1)]
    vsq = [ar.alloc([SEQ // 128, 2, 128], BF16) for _ in range(1)]
    memset("pool", vsq[0][0][:, :, :, 64:128], 1.0, [vsq[0][1]])
    alb, albB = ar.alloc([2, 16, 256], BF16)
    for hl in range(2):
        for hh_ in range(4):
            dma("pool", alb[:, hl, 4 * hh_:4 * hh_ + 4, :], alibi2[:, hl, 4 * hh_:4 * hh_ + 4, :], writes=[albB])
    qTt = [ar.alloc([16, 512], BF16, parts=64) for _ in range(2)]
    sA = [ar.alloc([512], F32) for _ in range(2)]
    pA_ = [ar.alloc([512], BF16) for _ in range(2)]
    sB = [ar.alloc([512], F32) for _ in range(2)]
    pB_ = [ar.alloc([512], BF16) for _ in range(2)]
    dn_t, dnB_ = ar.alloc([512], F32)
    dns = [ar.alloc([512], F32) for _ in range(2)]
    aos = [ar.alloc([512], BF16) for _ in range(2)]
    ckf = [ar.alloc([128], BF16) for _ in range(2)]
    kTc, kTcB = ar.alloc([2, 128], BF16, parts=64)
    kTn, kTnB = ar.alloc([2, 64], BF16, parts=64)
    vn, vnB = ar.alloc([128], BF16, parts=64)
    qTs_, qTsB = ar.alloc([16, 64], BF16, parts=64)
    ui = 0
    for sq in range(2):
        (kT, kTB), (vs, vsB) = kTq[0], vsq[0]
        g0s = sq * SEQ
        dma("sync", kT[0:64, :, :], kct[:, :, g0s:g0s + SEQ].rearrange("h d t -> d h t"), reads=[B_kct], writes=[kTB])
        for kvh_ in range(2):
            dma("sync", vs[:, :, kvh_, 0:64], vc[g0s:g0s + SEQ, kvh_ * 64:(kvh_ + 1) * 64].rearrange("(k p) f -> p k f", p=128),
                reads=[B_vc], writes=[vsB])
        for qg in range(SEQ // 512):
            qT, qTB = qTt[qg % 2]
            dma("sync", qT[0:64, :, :], qct[:, :, g0s + qg * 512:g0s + (qg + 1) * 512].rearrange("h d t -> d h t"),
                reads=[B_qct], writes=[qTB])
            units = [(qq, grp) for qq in range(4) for grp in range(4)]

            def swa_s1(u, ui_):
                qq, grp = units[u]
                qb = qg * 4 + qq
                kvh = grp // 2
                a = ui_ % 2
                bA, bB, bO = (0, 1, 2) if a == 0 else (4, 5, 6)
                rhs = qT[0:64, 4 * grp:4 * grp + 4, qq * 128:(qq + 1) * 128]
                for (bk, kb_, c0_, pt) in ((bA, qb, 0, pA_[a]), (bB, qb - 1, 128, pB_[a])):
                    if kb_ < 0:
                        continue
                    mm(pbanks[bk][:, :], kT[0:64, kvh, kb_ * 128:(kb_ + 1) * 128], rhs, True, False, [kTB, qTB], [PB[bk]])
                    mm(pbanks[bk][:, :], cb[:, C_ID, :], alb[:, 0, 4 * grp:4 * grp + 4, c0_:c0_ + 128], False, False, [B_cb, albB], [PB[bk]])
                    mm(pbanks[bk][:, :], cb[:, C_ID, :], alb[:, 1, 4 * grp:4 * grp + 4, c0_:c0_ + 128], False, True, [B_cb, albB], [PB[bk]])
                    act(pt[0], pbanks[bk][:, :], AF.Exp, [PB[bk]], [pt[1]])

            def swa_s2(u, ui_):
                qq, grp = units[u]
                qb = qg * 4 + qq
                kvh = grp // 2
                a = ui_ % 2
                bA, bB, bO = (0, 1, 2) if a == 0 else (4, 5, 6)
                (p_a, p_aB), (p_b, p_bB) = pA_[a], pB_[a]
                mm(pbanks[bO][:, :], vs[:, qb, kvh, :], p_a, True, qb == 0, [vsB, p_aB], [PB[bO]])
                if qb > 0:
                    mm(pbanks[bO][:, :], vs[:, qb - 1, kvh, :], p_b, False, True, [vsB, p_bB], [PB[bO]])
                dn_, dnB2 = dns[a]
                for hh in range(4):
                    act(dn_[64:128, hh * 128:(hh + 1) * 128], pbanks[bO][64:128, hh * 128:(hh + 1) * 128], AF.Ln,
                        [PB[bO], eskB], [dnB2], bias=esk[64:128, 4 * grp + hh:4 * grp + hh + 1])
                ao, aoB = aos[a]
                act(dn_[64:128, :], dn_[64:128, :], AF.Exp, [dnB2], [dnB2], scale=-1.0)
                tt("dve", ao[0:64, :], pbanks[bO][0:64, :], dn_[64:128, :], ALU.mult, [PB[bO], dnB2], [aoB])
                dma("sync", ao_c[4 * grp:4 * grp + 4, :, g0s + qb * 128:g0s + (qb + 1) * 128].rearrange("h d t -> d h t"),
                    ao[0:64, :].rearrange("p (h t) -> p h t", h=4), reads=[aoB], writes=[B_aoc])

            for u in range(len(units) + 1):
                if u < len(units):
                    swa_s1(u, ui + u)
                if u >= 1:
                    swa_s2(u - 1, ui + u - 1)
            ui += len(units)
    for s2 in range(2):
        g0s = 2 * SEQ + s2 * 64
        (ck, ckB), (cv, cvB) = ckf[0], ckf[1]
        dma("pool", ck, c_wk[s2], writes=[ckB])
        dma("pool", cv, c_wv[s2], writes=[cvB])
        ptb = pbanks[7][:].bitcast(BF16)
        for kvh in range(2):
            tr(ptb[0:64, kvh * 128:(kvh + 1) * 128], ck[:, kvh * 64:(kvh + 1) * 64], cb[:, C_ID, :], [ckB, B_cb], [PB[7]])
        cp("act", kTc[0:64, :, :], ptb[0:64, 0:256].rearrange("p (h t) -> p h t", h=2), [PB[7]], [kTcB])
        dma("sync", kTn[0:64, :, :], kct[:, :, g0s:g0s + 64].rearrange("h d t -> d h t"), reads=[B_kct], writes=[kTnB])
        dma("sync", vn[0:64, :], vc[g0s:g0s + 64, :], reads=[B_vc], writes=[vnB])
        dma("sync", qTs_[0:64, :, :], qct[:, :, g0s:g0s + 64].rearrange("h d t -> d h t"), reads=[B_qct], writes=[qTsB])
        for grp in range(4):
            kvh = grp // 2
            a = ui % 2; ui += 1
            bA, bB, bO, bD = (0, 1, 2, 3) if a == 0 else (4, 5, 6, 7)
            rhs = qTs_[0:64, 4 * grp:4 * grp + 4, :]
            (s_a, s_aB), (p_a, p_aB) = sA[a], pA_[a]
            (s_b, s_bB), (p_b, p_bB) = sB[a], pB_[a]
            mm(pbanks[bA][0:64, 0:256], kTn[0:64, kvh, :], rhs, True, True, [kTnB, qTsB], [PB[bA]])
            tt("dve", s_a[0:64, 0:256].rearrange("p (h t) -> p h t", h=4), pbanks[bA][0:64, 0:256].rearrange("p (h t) -> p h t", h=4),
               alib[0:64, 4 * grp:4 * grp + 4, 0:64], ALU.add, [PB[bA], alibB], [s_aB])
            act(p_a[0:64, 0:256], s_a[0:64, 0:256], AF.Exp, [s_aB], [p_aB])
            mm(pbanks[bB][:, 0:256], kTc[0:64, kvh, :], rhs, True, True, [kTcB, qTsB], [PB[bB]])
            tt("dve", s_b[:, 0:256].rearrange("p (h t) -> p h t", h=4), pbanks[bB][:, 0:256].rearrange("p (h t) -> p h t", h=4),
               alib[:, 4 * grp:4 * grp + 4, 128:192], ALU.add, [PB[bB], alibB], [s_bB])
            act(p_b[:, 0:256], s_b[:, 0:256], AF.Exp, [s_bB], [p_bB])
            mm(pbanks[bO][0:64, 0:256], vn[0:64, kvh * 64:(kvh + 1) * 64], p_a[0:64, 0:256], True, False, [vnB, p_aB], [PB[bO]])
            mm(pbanks[bO][0:64, 0:256], cv[:, kvh * 64:(kvh + 1) * 64], p_b[:, 0:256], False, True, [cvB, p_bB], [PB[bO]])
            mm(pbanks[bD][0:64, 0:256], cb[0:64, C_ONE, 0:64], p_a[0:64, 0:256], True, False, [B_cb, p_aB], [PB[bD]])
            mm(pbanks[bD][0:64, 0:256], cb[:, C_ONE, 0:64], p_b[:, 0:256], False, True, [B_cb, p_bB], [PB[bD]])
            for hh in range(4):
                ts("dve", dn_t[0:64, hh * 64:(hh + 1) * 64], pbanks[bD][0:64, hh * 64:(hh + 1) * 64],
                   esk[0:64, 4 * grp + hh:4 * grp + hh + 1], ALU.add, [PB[bD], eskB], [dnB_])
            recip(dn_t[0:64, 0:256], dn_t[0:64, 0:256], [dnB_], [dnB_])
            ao, aoB = aos[a]
            tt("dve", ao[0:64, 0:256], pbanks[bO][0:64, 0:256], dn_t[0:64, 0:256], ALU.mult, [PB[bO], dnB_], [aoB])
            dma("sync", ao_c[4 * grp:4 * grp + 4, :, g0s:g0s + 64].rearrange("h d t -> d h t"),
                ao[0:64, 0:256].rearrange("p (h t) -> p h t", h=4), reads=[aoB], writes=[B_aoc])
    P.barrier()
    if stop_after == 'D2':
        return nc, P, st, locals()

    def ao_loader1(tile, buf):
        kind, sq, t0, TW, g0 = tile
        t, b = buf
        dma("sync", t[:, :, 0:TW], ao_view(ao_c)[:, :, g0:g0 + TW], reads=[B_aoc], writes=[b])

    def x_loader1(tile, buf):
        kind, sq, t0, TW, g0 = tile
        t, b = buf
        dma("sync", t[:, 0:TW // 128, :], x2[g0:g0 + TW, :].rearrange("(s p) d -> p s d", p=128), reads=[B_x2], writes=[b])

    phase_outproj("moe", ao_loader1, x_loader1, w_out_c, g_moe, x3, B_x3, h4t, B_h4t, router=True)
    if stop_after == 'D3':
        return nc, P, st, locals()

    B_ind = Buf("indirect_chain")
    import os as _os2
    CHAIN = _os2.environ.get("CHAIN", "1") == "1"

    B_ind2 = [Buf("indirect_chain0"), Buf("indirect_chain1")]
    ind_ctr = [0]
    NCHAIN = int(_os2.environ.get("NCHAIN", "2"))

    def chain_buf():
        b = B_ind2[ind_ctr[0] % NCHAIN]
        ind_ctr[0] += 1
        return b

    def idma_gather(out, table, idx_ap, reads, writes):
        if CHAIN:
            cbuf = chain_buf()
            reads = list(reads) + [cbuf]
            writes = list(writes) + [cbuf]
        return P.add("pool", lambda e, o=out, t=table, ix=idx_ap: e.indirect_dma_start(
            out=o, out_offset=None, in_=t, in_offset=bass.IndirectOffsetOnAxis(ap=ix, axis=0)), reads, writes, is_dma=True)

    def idma_scatter(table, idx_ap, in_, reads, writes):
        if CHAIN:
            cbuf = chain_buf()
            reads = list(reads) + [cbuf]
            writes = list(writes) + [cbuf]
        return P.add("pool", lambda e, o=table, s_=in_, ix=idx_ap: e.indirect_dma_start(
            out=o, out_offset=bass.IndirectOffsetOnAxis(ap=ix, axis=0), in_=s_, in_offset=None), reads, writes, is_dma=True)

    ar.reset()
    c8 = {k: ar.alloc([NEXP], F32) for k in ("t", "m", "pad", "pend", "pst")}
    mm(pbanks[6][:, 16:24], cf[:, C_ONE, :], spm, True, True, [B_cf, spmB], [PB[6]])
    cp("dve", c8["t"][0], pbanks[6][:, 16:24], [PB[6]], [c8["t"][1]])
    t48, t48B = ar.alloc([48], F32)
    for e in range(NEXP):
        ts("dve", t48, iota_t[:, 8:56], c8["t"][0][:, e:e + 1], ALU.subtract, [B_iota, c8["t"][1]], [t48B], s2=-1.0, op1=ALU.mult)
        ts("dve", t48, t48, 0.0, ALU.max, [t48B], [t48B], s2=1.0, op1=ALU.min)
        P.add("dve", lambda e_, o=c8["m"][0][:, e:e + 1], i=t48: e_.tensor_reduce(out=o, in_=i, axis=AX, op=ALU.add), [t48B], [c8["m"][1]])
    ts("dve", c8["pad"][0], c8["m"][0], 512.0, ALU.mult, [c8["m"][1]], [c8["pad"][1]])
    pend, pendB = c8["pend"]
    pad_, padB = c8["pad"]
    cp("dve", pend[:, 0:1], pad_[:, 0:1], [padB], [pendB])
    for e in range(1, NEXP):
        tt("dve", pend[:, e:e + 1], pend[:, e - 1:e], pad_[:, e:e + 1], ALU.add, [pendB, padB], [pendB])
    pst, pstB = c8["pst"]
    tt("dve", pst, pend, pad_, ALU.subtract, [pendB, padB], [pstB])
    for st_ in range(NT):
        tt("dve", RANK[:, st_, :], RANK[:, st_, :], pst, ALU.add, [B_RK, pstB], [B_RK])
    TMP, TMPB = ar.alloc([NT, NEXP], F32)
    slf = [ar.alloc([NT], F32) for _ in range(2)]
    for k, (MK, B_MK) in enumerate(((MK1, B_MK1), (MK2, B_MK2))):
        tt("dve", TMP, MK, RANK, ALU.mult, [B_MK, B_RK], [TMPB])
        P.add("dve", lambda e, o=slf[k][0], i=TMP: e.tensor_reduce(out=o, in_=i, axis=AX, op=ALU.add), [TMPB], [slf[k][1]])
        cp("dve", SL[:, :, k], slf[k][0], [slf[k][1]], [B_SL])
    eb, ebB = ar.alloc([48], F32)
    memset("dve", eb, 0.0, [ebB])
    for e in range(NEXP):
        ts("dve", t48, iota_t[:, 8:56], pend[:, e:e + 1], ALU.subtract, [B_iota, pendB], [t48B], s2=1.0, op1=ALU.add)
        ts("dve", t48, t48, 0.0, ALU.max, [t48B], [t48B], s2=1.0, op1=ALU.min)
        tt("dve", eb, eb, t48, ALU.add, [ebB, t48B], [ebB])
    ts("dve", eb, eb, float(NEXP - 1), ALU.min, [ebB], [ebB])
    IDXf, IDXfB = ar.alloc([NBLK, 4], F32)
    IDX, B_IDX = ar.alloc([NBLK, 4], I32)
    for q in range(4):
        ts("dve", IDXf[:, :, q], eb[:, 0:NBLK], 512.0, ALU.mult, [ebB], [IDXfB], s2=q * 128.0, op1=ALU.add)
        ts("dve", IDXf[:, :, q], IDXf[:, :, q], iota_t[:, 56:57], ALU.add, [IDXfB, B_iota], [IDXfB])
    cp("dve", IDX, IDXf, [IDXfB], [B_IDX])
    if stop_after == 'E0':
        dbg = y_s.rearrange("a q d -> (a q) d")
        dma("sync", dbg[:, 0:NT], slf[0][0], reads=[slf[0][1]], writes=[OUTB])
        dma("sync", dbg[:, 16:16 + NT], slf[1][0], reads=[slf[1][1]], writes=[OUTB])
        dma("sync", dbg[:, 32:80], eb, reads=[ebB], writes=[OUTB])
        dma("sync", dbg[:, 80:88], pend, reads=[pendB], writes=[OUTB])
        dma("sync", dbg[:, 88:96], c8["t"][0], reads=[c8["t"][1]], writes=[OUTB])
        dma("sync", dbg[:, 100:100 + NBLK * 4], IDXf.rearrange("p b q -> p (b q)"), reads=[IDXfB], writes=[OUTB])
        dma("sync", dbg[:, 200:200 + NT * 2], SL.rearrange("p a b -> p (a b)").bitcast(F32), reads=[B_SL], writes=[OUTB])
        dma("sync", dbg[:, 300:300 + NBLK * 4], IDX.rearrange("p a b -> p (a b)").bitcast(F32), reads=[B_IDX], writes=[OUTB])
        P.barrier()
        return nc, P, st, locals()
    xks = [ar.alloc([4, D], BF16) for _ in range(2)]
    z8, z8B = xks[0]
    B_xz = Buf("xsl_zero", multi=True)
    memset("dve", z8, 0.0, [z8B])
    for b_ in range(NBLK):
        dma("sync", xsl[b_ * 512:(b_ + 1) * 512, :].rearrange("(s p) d -> p s d", p=128), z8, reads=[z8B], writes=[B_xz])
    hks = [ar.alloc([D], BF16) for _ in range(2)]
    for st_ in range(NT):
        hk, hkB = hks[st_ % 2]
        dma("sync", hk, h4k[st_ * 128:(st_ + 1) * 128, :], reads=[B_h4k], writes=[hkB])
        for k in range(2):
            idma_scatter(xsl[:, :], SL[:, st_, k:k + 1], hk, [B_SL, hkB, B_xz], [B_xsl])
    if stop_after == 'E1':
        P.barrier()
        return nc, P, st, locals()
    wq = [(ar.alloc([8, 896], BF16), ar.alloc([8, 896], BF16), ar.alloc([7, D], BF16)) for _ in range(2)]
    xTs = [ar.alloc([8, 512], BF16) for _ in range(2)]
    aTs = [ar.alloc([7, 512], BF16) for _ in range(2)]
    sgs = [ar.alloc([512], F32) for _ in range(2)]
    yss = [ar.alloc([4, D], F32) for _ in range(2)]

    def ld_blk(b_):
        dma("sync", xks[b_ % 2][0], xsl[b_ * 512:(b_ + 1) * 512, :].rearrange("(s p) d -> p s d", p=128), reads=[B_xsl], writes=[xks[b_ % 2][1]])

    def ld_w(j):
        b_, q = j // 4, j % 4
        (wg, wgB), (wu, wuB), (wd, wdB) = wq[j % 2]
        idma_gather(wg.rearrange("p k m -> p (k m)"), wge_s[:, :], IDX[:, b_, q:q + 1], [B_IDX, B_wge], [wgB])
        idma_gather(wu.rearrange("p k m -> p (k m)"), wue_s[:, :], IDX[:, b_, q:q + 1], [B_IDX, B_wue], [wuB])
        idma_gather(wd.rearrange("p k m -> p (k m)"), wde_s[:, :], IDX[:, b_, q:q + 1], [B_IDX, B_wde], [wdB])

    ld_blk(0)
    ld_w(0)
    for b_ in range(NBLK):
        if b_ + 1 < NBLK:
            ld_blk(b_ + 1)
        xk, xkB = xks[b_ % 2]
        xT, xTB = xTs[b_ % 2]
        ys, ysB = yss[b_ % 2]
        for s in range(4):
            transpose_to(xk[:, s, :], xkB, xT, xTB, s * 128, 128, 7)
        for q in range(4):
            j = b_ * 4 + q
            if j + 1 < NBLK * 4:
                ld_w(j + 1)
            (wg, wgB), (wu, wuB), (wd, wdB) = wq[j % 2]
            aT, aTB = aTs[j % 2]
            gate_up(xT, xTB, 512, wg, wgB, wu, wuB, 7, aT, aTB, sgs)
            for s in range(4):
                for half in range(2):
                    pbi = 4 + (2 * s + half) % 3
                    for m in range(7):
                        mm(pbanks[pbi][:, :], aT[:, m, s * 128:(s + 1) * 128], wd[:, m, half * 512:(half + 1) * 512],
                           m == 0, m == 6, [aTB, wdB], [PB[pbi]])
                    ysl_ = ys[:, s, half * 512:(half + 1) * 512]
                    if q == 0:
                        cp("dve", ysl_, pbanks[pbi][:, :], [PB[pbi]], [ysB])
                    else:
                        tt("dve", ysl_, pbanks[pbi][:, :], ysl_, ALU.add, [PB[pbi], ysB], [ysB])
        dma("sync", ysl[b_ * 512:(b_ + 1) * 512, :].rearrange("(s p) d -> p s d", p=128), ys, reads=[ysB], writes=[B_ysl])
    P.barrier()
    ar.reset()
    load_gain("fin", g_fin)
    x3s = [ar.alloc([D], F32) for _ in range(2)]
    y1s = [ar.alloc([D], F32) for _ in range(2)]
    y2s = [ar.alloc([D], F32) for _ in range(2)]
    yo = [ar.alloc([D], F32) for _ in range(2)]
    tmp = (ar.alloc([D], F32), ar.alloc([1], F32), ar.alloc([1], F32))
    subt = []
    for (kind, sq, t0, TW, g0) in tiles:
        for s in range(TW // 128):
            subt.append((kind, sq, t0 + s * 128))
    assert len(subt) == NT

    def ld_e3(st_):
        dma("sync", x3s[st_ % 2][0], x3[st_ * 128:(st_ + 1) * 128, :], reads=[B_x3], writes=[x3s[st_ % 2][1]])
        idma_gather(y1s[st_ % 2][0], ysl[:, :], SL[:, st_, 0:1], [B_SL, B_ysl], [y1s[st_ % 2][1]])
        idma_gather(y2s[st_ % 2][0], ysl[:, :], SL[:, st_, 1:2], [B_SL, B_ysl], [y2s[st_ % 2][1]])

    ld_e3(0)
    for st_, (kind, sq, r0) in enumerate(subt):
        if st_ + 1 < NT:
            ld_e3(st_ + 1)
        (xt, xB), (y1, y1B), (y2, y2B) = x3s[st_ % 2], y1s[st_ % 2], y2s[st_ % 2]
        stt(xt, y1, G12[:, st_, 0:1], xt, ALU.mult, ALU.add, [y1B, B_G12, xB], [xB])
        stt(xt, y2, G12[:, st_, 1:2], xt, ALU.mult, ALU.add, [y2B, B_G12, xB], [xB])
        (sq_, sqB), (ss, ssB), (rs, rsB) = tmp
        g = gtile["fin"]
        act(sq_, xt, AF.Square, [xB], [sqB, ssB], accum=ss)
        act(rs, ss, AF.Ln, [ssB], [rsB], scale=1.0 / D, bias=EPS)
        act(rs, rs, AF.Exp, [rsB], [rsB], scale=-0.5)
        yo_, yoB = yo[st_ % 2]
        stt(yo_, xt, rs[:, 0:1], g[0], ALU.mult, ALU.mult, [xB, rsB, g[1]], [yoB])
        if kind == "p":
            dma("sync", y_p[sq, r0:r0 + 128, :], yo_, reads=[yoB], writes=[OUTB])
        else:
            dma("sync", y_s[0], yo_[0:64, :], reads=[yoB], writes=[OUTB])
            dma("sync", y_s[1], yo_[64:128, :], reads=[yoB], writes=[OUTB])
    P.barrier()
    return nc, P, st, locals()


def make_consts():
    j = np.arange(128)[:, None]
    s = np.arange(128)[None, :]
    c = np.zeros((128, 14, 128), np.float32)
    c[:, 0] = (j == s)
    c[:, 1] = -(j >= s).astype(np.float32)
    c[:, 2] = (j < s)
    c[:, 3] = (j <= s)
    c[:, 4] = np.where(j < s, 0.0, NEGBIG)
    c[:, 5] = np.where(j <= s, 0.0, NEGBIG)
    c[:, 6] = (j <= s)
    c[:, 7] = 1.0
    c[:, 8] = (s < 64) * np.ones((128, 1))
    c[:, 9] = (s >= 64) * np.ones((128, 1))
    c[:, 10] = (j < s)
    c[:, 11] = 0.0
    c[:, 11, 64] = -1.0
    c[:, 12] = (j <= s) & ((j < 64) == (s < 64))
    c[:, 13] = (j == 0) * np.ones((1, 128))
    return c


def make_alibi():
    sk = np.arange(128)[:, None]
    tq = np.arange(256)[None, :]
    vis = ((tq // 64) - (sk // 64) >= 0) & ((tq // 64) - (sk // 64) <= 2)
    dist = np.abs(tq - sk).astype(np.float32)
    out = np.zeros((128, 16, 256), np.float32)
    for h in range(16):
        slope = np.float32(2.0 ** (-8.0 * (h + 1.0) / 16))
        out[:, h, :] = np.where(vis, -slope * dist, NEGBIG)
    return out


def make_iota():
    t = np.zeros((128, 64), np.float32)
    t[:, 0:8] = np.arange(8)[None, :] * 128 + np.arange(128)[:, None]
    t[:, 8:56] = np.arange(48)[None, :] * 512.0
    t[:, 56] = np.arange(128)
    return t


_CACHE = {}


def run(inputs, SEQ, PAST, ncores=8):
    key = (SEQ, PAST)
    if key not in _CACHE:
        import os
        nc, P, st, _ = build(SEQ, PAST, os.environ.get('STOP'))
        P.emit()
        _CACHE[key] = nc
    nc = _CACHE[key]
    f = lambda a: np.ascontiguousarray(np.asarray(a, dtype=np.float32))
    I = {k: np.asarray(v) for k, v in inputs.items()}
    _al = make_alibi()
    _hi = _al.astype(BF16NP).astype(np.float32)
    consts = {"cst": make_consts(), "alibi": _al, "alibi2": np.ascontiguousarray(np.stack([_hi, _al - _hi], axis=1)), "iot": make_iota()}
    shared = {
        "g_ab": f(I["norm_mix_ab"][0:1]), "w_in_ab": f(I["w_in_ab"][0]), "b_fg": f(I["b_forget"][0:1]),
        "w_out_ab": f(I["w_out_ab"][0]), "g_ffn": f(I["norm_ffn_dense"][0:1]),
        "w_g": f(I["w_gate_dense"][0]), "w_u": f(I["w_up_dense"][0]), "w_d": f(I["w_down_dense"][0]),
        "g_c": f(I["norm_mix_c"][0:1]), "w_in_c": f(I["w_in_c"][0]), "sinks": f(I["sinks"][0:1]),
        "w_out_c": f(I["w_out_c"][0]), "g_moe": f(I["norm_ffn_moe"][0:1]), "w_r": f(I["w_router"][0]),
        "w_ge": f(I["w_gate_moe"][0]), "w_ue": f(I["w_up_moe"][0]), "w_de": f(I["w_down_moe"][0]),
        "g_fin": f(I["norm_final"][None, :]),
    }
    shared.update(consts)
    in_maps = []
    for c in range(ncores):
        sl = slice(2 * c, 2 * c + 2)
        m = dict(shared)
        m["xp"] = f(I["x_prompt"][sl]); m["xs"] = f(I["x_sample"][sl])
        m["c_sbk"] = f(I["cache_sb_k"][0, sl]).reshape(2, PAST, 512)
        m["c_sbv"] = f(I["cache_sb_v"][0, sl]).reshape(2, PAST, 512)
        m["c_fxk"] = f(I["cache_fox_k"][0, sl]).reshape(2, PAST, 512)
        m["c_fxv"] = f(I["cache_fox_v"][0, sl]).reshape(2, PAST, 512)
        m["c_lf"] = f(I["cache_fox_logf"][0, sl])
        m["c_wk"] = f(I["cache_swa_k"][0, sl]).reshape(2, 128, 128)
        m["c_wv"] = f(I["cache_swa_v"][0, sl]).reshape(2, 128, 128)
        in_maps.append(m)
    res = run_bass_kernel_spmd(nc, in_maps, core_ids=list(range(ncores)))
    R = res.results
    cat = lambda k: np.concatenate([np.asarray(r[k]) for r in R], axis=0)
    B = 2 * ncores
    outs = (
        cat("y_p"), cat("y_s"),
        cat("p_sbk").reshape(1, B, SEQ, 8, 64), cat("p_sbv").reshape(1, B, SEQ, 8, 64),
        cat("p_fxk").reshape(1, B, SEQ, 8, 64), cat("p_fxv").reshape(1, B, SEQ, 8, 64),
        cat("p_lf").reshape(1, B, SEQ, 8),
        cat("p_wk").reshape(1, B, 128, 2, 64), cat("p_wv").reshape(1, B, 128, 2, 64),
        cat("s_sbk").reshape(1, B, NQ, 8, 64), cat("s_sbv").reshape(1, B, NQ, 8, 64),
        cat("s_fxk").reshape(1, B, NQ, 8, 64), cat("s_fxv").reshape(1, B, NQ, 8, 64),
        cat("s_lf").reshape(1, B, NQ, 8),
        cat("s_wk").reshape(1, B, 128, 2, 64), cat("s_wv").reshape(1, B, 128, 2, 64),
    )
    return tuple(np.ascontiguousarray(o, dtype=np.float32) for o in outs)


def kernel(**inputs):
    return run(inputs, 4096, 4096, 8)
```
